# Optimizing a Trainium2 kernel written in Bass

```python
import math
import jax, jax.numpy as jnp
from jax import lax
import numpy as np

D_MODEL = 4096
BATCH = 4
SEQ = 4096
DEPTH = 1

M_HEADS = 8
M_QK_DIM = D_MODEL // (2 * M_HEADS)
M_V_DIM = D_MODEL // M_HEADS
M_QK = M_HEADS * M_QK_DIM
M_V = M_HEADS * M_V_DIM
M_CHUNK = 64
CONV_WIDTH = 3

A_HEADS = D_MODEL // 128
A_NOPE = 128
A_ROPE = 64
A_V = 128
Q_LORA = D_MODEL // 4
KV_LORA = 512
ROPE_THETA = 10000.0
Q_BLOCK = 128
ATTN_SCALE = (A_NOPE + A_ROPE) ** -0.5

N_GROUPS = 8
EXPERTS_PER_GROUP = 4
N_EXPERTS = N_GROUPS * EXPERTS_PER_GROUP
TOP_K = 2
D_EXPERT = D_MODEL // 4

LN_EPS = 1e-5
RMS_EPS = 1e-6
DEEPNORM_ALPHA = (2 * DEPTH) ** 0.25
DEEPNORM_BETA = (8 * DEPTH) ** -0.25

IN_SIZES = (M_QK, M_QK, M_V, M_V, 4 * M_HEADS, Q_LORA, KV_LORA, A_ROPE, D_MODEL, D_MODEL)

kernel_name = "hybrid_mlstm_mla_hmoe_deepnorm_encoder"


def layer_norm(x, g, b):
    xf = x.astype(jnp.float32)
    mu = jnp.mean(xf, -1, keepdims=True)
    xc = xf - mu
    var = jnp.mean(xc * xc, -1, keepdims=True)
    return (xc * lax.rsqrt(var + LN_EPS) * g.astype(jnp.float32) + b.astype(jnp.float32)).astype(x.dtype)


def rms_norm(x, g):
    xf = x.astype(jnp.float32)
    return (xf * lax.rsqrt(jnp.mean(xf * xf, -1, keepdims=True) + RMS_EPS) * g.astype(jnp.float32)).astype(x.dtype)


def rope_tables(seq, dim):
    inv = 1.0 / (ROPE_THETA ** (jnp.arange(0, dim, 2, dtype=jnp.float32) / dim))
    ang = jnp.arange(seq, dtype=jnp.float32)[:, None] * inv[None, :]
    return jnp.cos(ang), jnp.sin(ang)


def apply_rope(x, cos, sin):
    xf = x.astype(jnp.float32)
    x1, x2 = jnp.split(xf, 2, axis=-1)
    return jnp.concatenate([x1 * cos - x2 * sin, x1 * sin + x2 * cos], -1).astype(x.dtype)


def centred_depthwise_conv(x, w):
    pad = CONV_WIDTH // 2
    return lax.conv_general_dilated(
        x, w[:, None, :].astype(x.dtype), window_strides=(1,), padding=[(pad, pad)],
        dimension_numbers=('NWC', 'WIO', 'NWC'), feature_group_count=x.shape[-1])


def mlstm_chunkwise(q, k, v, log_i, log_f):
    B, H, S, dk = q.shape
    dv = v.shape[-1]
    L = M_CHUNK
    nc = S // L

    def to_chunks(t):
        return jnp.moveaxis(t.reshape((B, H, nc, L) + t.shape[3:]), 2, 0)

    xs = tuple(to_chunks(t) for t in (q, k, v, log_i, log_f))
    lower = jnp.tril(jnp.ones((L, L), dtype=bool))

    def step(carry, blk):
        C, n, m = carry
        qb, kb, vb, ib, fb = blk
        b = jnp.cumsum(fb, axis=-1)
        dmat = b[..., :, None] - b[..., None, :] + ib[..., None, :]
        dmat = jnp.where(lower, dmat, -jnp.inf)
        inter = b + m[..., None]
        m_t = jnp.maximum(inter, jnp.max(dmat, -1))
        w_inter = jnp.exp(inter - m_t)
        s_qk = jnp.einsum('bhtd,bhsd->bhts', qb, kb) * jnp.exp(dmat - m_t[..., None])
        num = w_inter[..., None] * jnp.einsum('bhtd,bhde->bhte', qb, C) + jnp.einsum('bhts,bhse->bhte', s_qk, vb)
        den = w_inter * jnp.einsum('bhtd,bhd->bht', qb, n) + jnp.sum(s_qk, -1)
        h = num / jnp.maximum(jnp.abs(den), jnp.exp(-m_t))[..., None]
        b_last = b[..., -1]
        dec = b_last[..., None] - b + ib
        m_new = jnp.maximum(b_last + m, jnp.max(dec, -1))
        a = jnp.exp(b_last + m - m_new)
        ws = jnp.exp(dec - m_new[..., None])
        C_new = a[..., None, None] * C + jnp.einsum('bhs,bhsd,bhse->bhde', ws, kb, vb)
        n_new = a[..., None] * n + jnp.einsum('bhs,bhsd->bhd', ws, kb)
        return (C_new, n_new, m_new), h

    init = (jnp.zeros((B, H, dk, dv), jnp.float32), jnp.zeros((B, H, dk), jnp.float32),
            jnp.zeros((B, H), jnp.float32))
    _, hc = lax.scan(step, init, xs)
    return jnp.moveaxis(hc, 0, 2).reshape(B, H, S, dv)


def mlstm_branch(q, k, v, o, gates, b_gates, conv_w, head_norm_g):
    B, S, _ = q.shape
    qk = jax.nn.silu(centred_depthwise_conv(jnp.concatenate([q, k], -1), conv_w))
    q, k = jnp.split(qk, 2, axis=-1)

    def heads(t, d):
        return t.reshape(B, S, M_HEADS, d).transpose(0, 2, 1, 3).astype(jnp.float32)

    qh = heads(q, M_QK_DIM)
    kh = heads(k, M_QK_DIM) * (M_QK_DIM ** -0.5)
    vh = heads(v, M_V_DIM)
    g = (gates.astype(jnp.float32) + b_gates.astype(jnp.float32)).reshape(B, S, 4, M_HEADS).transpose(2, 0, 3, 1)
    h_fwd = mlstm_chunkwise(qh, kh, vh, g[0], jax.nn.log_sigmoid(g[1]))
    flip = lambda t: jnp.flip(t, axis=2)
    h_bwd = flip(mlstm_chunkwise(flip(qh), flip(kh), flip(vh), flip(g[2]), flip(jax.nn.log_sigmoid(g[3]))))
    h = h_fwd + h_bwd
    h = h * lax.rsqrt(jnp.mean(h * h, -1, keepdims=True) + RMS_EPS)
    h = h.transpose(0, 2, 1, 3).reshape(B, S, M_V) * head_norm_g.astype(jnp.float32)
    return (jax.nn.sigmoid(o.astype(jnp.float32)) * h).astype(o.dtype)


def mla_branch(c_q, c_kv, k_r, q_norm_g, w_uq, kv_norm_g, w_ukv, cos, sin):
    B, S, _ = c_q.shape
    q = (rms_norm(c_q, q_norm_g) @ w_uq).reshape(B, S, A_HEADS, A_NOPE + A_ROPE).transpose(0, 2, 1, 3)
    q_nope = q[..., :A_NOPE]
    q_rope = apply_rope(q[..., A_NOPE:], cos, sin)
    kv = (rms_norm(c_kv, kv_norm_g) @ w_ukv).reshape(B, S, A_HEADS, A_NOPE + A_V).transpose(0, 2, 1, 3)
    k_nope, v = kv[..., :A_NOPE], kv[..., A_NOPE:]
    k_rope = apply_rope(k_r, cos, sin)
    n_blocks = S // Q_BLOCK

    def to_blocks(t):
        return t.reshape(B, A_HEADS, n_blocks, Q_BLOCK, t.shape[-1]).transpose(2, 0, 1, 3, 4)

    def attend(blk):
        qn, qr = blk
        s = (jnp.einsum('bhqd,bhkd->bhqk', qn, k_nope).astype(jnp.float32)
             + jnp.einsum('bhqd,bkd->bhqk', qr, k_rope).astype(jnp.float32)) * ATTN_SCALE
        p = jax.nn.softmax(s, axis=-1).astype(v.dtype)
        return jnp.einsum('bhqk,bhkd->bhqd', p, v)

    o = lax.map(attend, (to_blocks(q_nope), to_blocks(q_rope)))
    return o.transpose(1, 0, 3, 2, 4).reshape(B, S, A_HEADS * A_V)


def hier_moe(x, w_rg, b_rg, w_re, b_re, w_gate, w_up, w_down):
    B, S, D = x.shape
    T = B * S
    xt = x.reshape(T, D)
    g_logits = (xt @ w_rg).astype(jnp.float32) + b_rg.astype(jnp.float32)
    g_prob = jax.nn.softmax(g_logits, axis=-1)
    g_top, g_idx = lax.top_k(g_prob, 1)
    e_logits = ((xt @ w_re).astype(jnp.float32) + b_re.astype(jnp.float32)).reshape(T, N_GROUPS, EXPERTS_PER_GROUP)
    e_in = jnp.take_along_axis(e_logits, g_idx[:, :, None], axis=1)[:, 0]
    top_v, top_i = lax.top_k(e_in, TOP_K)
    top_w = jax.nn.softmax(top_v, axis=-1) * g_top
    eid = g_idx * EXPERTS_PER_GROUP + top_i
    combine = jnp.sum(jax.nn.one_hot(eid, N_EXPERTS, dtype=jnp.float32) * top_w[..., None], axis=1)

    def expert(acc, e):
        wg, wu, wd, c = e
        h = jax.nn.silu(xt @ wg) * (xt @ wu)
        return acc + c[:, None].astype(xt.dtype) * (h @ wd), None

    out, _ = lax.scan(expert, jnp.zeros_like(xt), (w_gate, w_up, w_down, combine.T))
    return out.reshape(B, S, D)


def setup_inputs(seed: int = 0) -> dict:
    key = jax.random.key(seed)
    ks = iter(jax.random.split(key, 64))
    f32 = jnp.float32
    Lr = DEPTH
    beta = DEEPNORM_BETA

    def nrm(shape, scale):
        return jax.random.normal(next(ks), shape, f32) * scale

    def gain(shape):
        return 1.0 + nrm(shape, 0.02)

    x = nrm((BATCH, SEQ, D_MODEL), 1.0)
    ln0_g = gain((D_MODEL,))
    ln0_b = nrm((D_MODEL,), 0.02)
    in_scales = (1.0, 1.0, beta, 1.0, 1.0, 1.0, 1.0, 1.0, 1.0, 1.0)
    w_in = jnp.concatenate([nrm((Lr, D_MODEL, n), D_MODEL ** -0.5 * s) for n, s in zip(IN_SIZES, in_scales)], axis=-1)
    i_bias = nrm((Lr, M_HEADS), 0.1)
    f_bias = jnp.linspace(3.0, 6.0, M_HEADS, dtype=f32) + nrm((Lr, M_HEADS), 0.1)
    i_bias_b = nrm((Lr, M_HEADS), 0.1)
    f_bias_b = jnp.linspace(3.0, 6.0, M_HEADS, dtype=f32) + nrm((Lr, M_HEADS), 0.1)
    b_gates = jnp.concatenate([i_bias, f_bias, i_bias_b, f_bias_b], axis=-1)
    conv_qk = nrm((Lr, CONV_WIDTH, 2 * M_QK), CONV_WIDTH ** -0.5)
    head_norm_g = gain((Lr, M_V))
    q_norm_g = gain((Lr, Q_LORA))
    w_uq = nrm((Lr, Q_LORA, A_HEADS * (A_NOPE + A_ROPE)), Q_LORA ** -0.5)
    kv_norm_g = gain((Lr, KV_LORA))
    w_uk = nrm((Lr, KV_LORA, A_HEADS, A_NOPE), KV_LORA ** -0.5)
    w_uv = nrm((Lr, KV_LORA, A_HEADS, A_V), KV_LORA ** -0.5 * beta)
    w_ukv = jnp.concatenate([w_uk, w_uv], axis=-1).reshape(Lr, KV_LORA, A_HEADS * (A_NOPE + A_V))
    w_pm = nrm((Lr, M_V, D_MODEL), M_V ** -0.5 * beta)
    w_pa = nrm((Lr, A_HEADS * A_V, D_MODEL), (A_HEADS * A_V) ** -0.5 * beta)
    w_out = nrm((Lr, D_MODEL, D_MODEL), D_MODEL ** -0.5 * beta)
    ln1_g = gain((Lr, D_MODEL))
    ln1_b = nrm((Lr, D_MODEL), 0.02)
    w_rg = nrm((Lr, D_MODEL, N_GROUPS), D_MODEL ** -0.5)
    b_rg = nrm((Lr, N_GROUPS), 0.01)
    w_re = nrm((Lr, D_MODEL, N_EXPERTS), D_MODEL ** -0.5)
    b_re = nrm((Lr, N_EXPERTS), 0.01)
    w_e_gate = nrm((Lr, N_EXPERTS, D_MODEL, D_EXPERT), D_MODEL ** -0.5)
    w_e_up = nrm((Lr, N_EXPERTS, D_MODEL, D_EXPERT), D_MODEL ** -0.5 * beta)
    w_e_down = nrm((Lr, N_EXPERTS, D_EXPERT, D_MODEL), D_EXPERT ** -0.5 * beta)
    ln2_g = gain((Lr, D_MODEL))
    ln2_b = nrm((Lr, D_MODEL), 0.02)
    return {"x": x, "ln0_g": ln0_g, "ln0_b": ln0_b, "w_in": w_in, "b_gates": b_gates,
            "conv_qk": conv_qk, "head_norm_g": head_norm_g, "q_norm_g": q_norm_g, "w_uq": w_uq,
            "kv_norm_g": kv_norm_g, "w_ukv": w_ukv, "w_pm": w_pm, "w_pa": w_pa, "w_out": w_out,
            "ln1_g": ln1_g, "ln1_b": ln1_b, "w_rg": w_rg, "b_rg": b_rg, "w_re": w_re, "b_re": b_re,
            "w_e_gate": w_e_gate, "w_e_up": w_e_up, "w_e_down": w_e_down, "ln2_g": ln2_g, "ln2_b": ln2_b}


def reference(x, ln0_g, ln0_b, w_in, b_gates, conv_qk, head_norm_g, q_norm_g, w_uq, kv_norm_g, w_ukv,
              w_pm, w_pa, w_out, ln1_g, ln1_b, w_rg, b_rg, w_re, b_re, w_e_gate, w_e_up, w_e_down,
              ln2_g, ln2_b):
    B, S, _ = x.shape
    split_points = [int(p) for p in np.cumsum(IN_SIZES)[:-1]]
    cos, sin = rope_tables(S, A_ROPE)
    x = layer_norm(x, ln0_g, ln0_b)
    for l in range(DEPTH):
        proj = x @ w_in[l]
        q_m, k_m, v_m, o_m, g_m, c_q, c_kv, k_r, gate_a, gate_b = jnp.split(proj, split_points, axis=-1)
        y_a = mlstm_branch(q_m, k_m, v_m, o_m, g_m, b_gates[l], conv_qk[l], head_norm_g[l]) @ w_pm[l]
        y_b = mla_branch(c_q, c_kv, k_r, q_norm_g[l], w_uq[l], kv_norm_g[l], w_ukv[l], cos, sin) @ w_pa[l]
        mixed = (jax.nn.sigmoid(gate_a) * y_a + jax.nn.sigmoid(gate_b) * y_b) @ w_out[l]
        x = layer_norm(DEEPNORM_ALPHA * x + mixed, ln1_g[l], ln1_b[l])
        moe = hier_moe(x, w_rg[l], b_rg[l], w_re[l], b_re[l], w_e_gate[l], w_e_up[l], w_e_down[l])
        x = layer_norm(DEEPNORM_ALPHA * x + moe, ln2_g[l], ln2_b[l])
    return x
```

```python
import numpy as np
from contextlib import ExitStack

import concourse.bass as bass
import concourse.mybir as mybir
from concourse.bass_utils import run_bass_kernel_spmd

F32 = mybir.dt.float32
BF16 = mybir.dt.bfloat16
AF = mybir.ActivationFunctionType
ALU = mybir.AluOpType

D = 4096
KC = 32
SEQ = 4096
TOWN = 2048
N_CORES = 8
LN_EPS = 1e-5
RMS_EPS = 1e-6
ALPHA = 2.0 ** 0.25

DEBUG_OUT = set()


class _Q:
    def __init__(self, name, sem):
        self.name, self.sem, self.count = name, sem, 0
        self.ops, self.seen, self.pr, self.pw = [], {}, [], []


class Sched:
    BLK = {"pe": "tensor", "act": "scalar", "dve": "vector", "pool": "gpsimd", "sp": "sync"}

    def __init__(self, nc, stack, n_dma_sems=24):
        self.nc = nc
        self.q = {n: _Q(n, stack.enter_context(nc.semaphore("s_" + n))) for n in self.BLK}
        self.dsem = [[stack.enter_context(nc.semaphore("d%d" % i)), 0] for i in range(n_dma_sems)]
        self.dnext = 0
        self.lw, self.rd = {}, {}
        self.nops = 0

    def _wait(self, q, ev):
        sem, val = ev
        if q.seen.get(id(sem), 0) >= val:
            return
        q.seen[id(sem)] = val
        q.ops.append(("wait", sem, val))

    def _deps(self, q, reads, writes, is_pe):
        for k in reads:
            ev = self.lw.get(k)
            if ev is not None and not (is_pe and ev[0] is q.sem):
                self._wait(q, ev)
        for k in writes:
            ev = self.lw.get(k)
            if ev is not None and not (is_pe and ev[0] is q.sem):
                self._wait(q, ev)
            for ev in self.rd.get(k, ()):
                if ev[0] is q.sem:
                    continue
                self._wait(q, ev)

    def _record(self, ev, reads, writes):
        for k in reads:
            lst = self.rd.setdefault(k, [])
            lst[:] = [e for e in lst if e[0] is not ev[0]]
            lst.append(ev)
        for k in writes:
            self.lw[k] = ev
            self.rd[k] = []

    def op(self, eng, fn, reads=(), writes=(), signal=True):
        q = self.q[eng]
        self._deps(q, reads, writes, eng == "pe")
        self.nops += 1
        if signal:
            q.count += 1
            ev = (q.sem, q.count)
            q.ops.append(("op", fn, ev))
            self._record(ev, list(reads) + q.pr, list(writes) + q.pw)
            q.pr, q.pw = [], []
        else:
            q.ops.append(("op", fn, None))
            q.pr += list(reads)
            q.pw += list(writes)

    def dma(self, queue, fn, reads=(), writes=()):
        q = self.q[queue]
        slot = self.dsem[self.dnext]
        self.dnext = (self.dnext + 1) % len(self.dsem)
        if slot[1]:
            self._wait(q, (slot[0], slot[1]))
        self._deps(q, reads, writes, False)
        slot[1] += 16
        ev = (slot[0], slot[1])
        q.ops.append(("dma", fn, slot[0]))
        self._record(ev, reads, writes)
        self.nops += 1

    def end_phase(self):
        sp = self.q["sp"]
        for sem, val in self.dsem:
            if val:
                self._wait(sp, (sem, val))
        for n, q in self.q.items():
            assert not q.pr and not q.pw, "unsignalled tail on " + n
            if n != "sp" and q.count:
                self._wait(sp, (q.sem, q.count))
        with self.nc.Block() as block:
            for n, q in self.q.items():
                if not q.ops:
                    continue

                def body(e, ops=q.ops):
                    for o in ops:
                        if o[0] == "wait":
                            e.wait_ge(o[1], o[2])
                        elif o[0] == "op":
                            inst = o[1](e)
                            if o[2] is not None:
                                inst.then_inc(o[2][0], 1)
                        else:
                            o[1](e).then_inc(o[2], 16)

                getattr(block, self.BLK[n])(body)
                q.ops = []
        self.lw, self.rd = {}, {}


class Ctx:
    def __init__(self, nc, S):
        self.nc, self.S = nc, S
        self.dram = {}

    def scratch(self, name, shape, dtype):
        kind = "ExternalOutput" if name in DEBUG_OUT else "Internal"
        t = self.nc.dram_tensor(name, list(shape), dtype, kind=kind)
        self.dram[name] = t
        return t.ap()


def bc_mid(ap2d, n):
    return ap2d.unsqueeze(1).to_broadcast([ap2d.shape[0], n, ap2d.shape[1]])


def ln_phase(cx, tag, n_tok, Tb, load_z, g_dram, b_dram, store_y):
    nc, S = cx.nc, cx.S
    nblk = n_tok // Tb
    with ExitStack() as st:
        sb = lambda n, shp, dt: st.enter_context(nc.sbuf_tensor(tag + n, shp, dt))
        ps = lambda n, shp, dt: st.enter_context(nc.psum_tensor(tag + n, shp, dt))
        ones = sb("ones", [128, 128], F32)
        gcol = sb("g", [128, KC], F32)
        bcol = sb("b", [128, KC], F32)
        z = [sb("z%d" % i, [128, KC, Tb], F32) for i in range(2)]
        sq = sb("sq", [128, KC, Tb], F32)
        y = [sb("y%d" % i, [128, KC, Tb], F32) for i in range(2)]
        mean = sb("mean", [128, Tb], F32)
        m2 = sb("m2", [128, Tb], F32)
        var = sb("var", [128, Tb], F32)
        rstd = sb("rstd", [128, Tb], F32)
        cc = sb("cc", [128, Tb], F32)
        s1 = [ps("s1_%d" % i, [128, Tb], F32) for i in range(2)]
        s2 = [ps("s2_%d" % i, [128, Tb], F32) for i in range(2)]

        S.op("dve", lambda e: e.memset(ones[:], 1.0), writes=["ones"])
        S.dma("sp", lambda e: e.dma_start(out=gcol[:], in_=g_dram), writes=["gcol"])
        S.dma("sp", lambda e: e.dma_start(out=bcol[:], in_=b_dram), writes=["bcol"])

        def front(blk):
            sl = blk % 2
            zk, zt = "z%d" % sl, z[sl]
            s1t, s2t, s1k, s2k = s1[sl], s2[sl], "s1_%d" % sl, "s2_%d" % sl
            load_z(blk, zt, zk)
            S.op("pool", lambda e: e.tensor_tensor(out=sq[:], in0=zt[:], in1=zt[:], op=ALU.mult),
                 reads=[zk], writes=["sq"])
            for kc in range(KC):
                S.op("pe", lambda e, kc=kc: e.matmul(s1t[:], lhsT=ones[:], rhs=zt[:, kc, :],
                                                     start=(kc == 0), stop=(kc == KC - 1)),
                     reads=[zk, "ones"], writes=[s1k], signal=(kc == KC - 1))
            for kc in range(KC):
                S.op("pe", lambda e, kc=kc: e.matmul(s2t[:], lhsT=ones[:], rhs=sq[:, kc, :],
                                                     start=(kc == 0), stop=(kc == KC - 1)),
                     reads=["sq", "ones"], writes=[s2k], signal=(kc == KC - 1))

        def back(blk):
            sl = blk % 2
            zk, yk = "z%d" % sl, "y%d" % sl
            zt, yt = z[sl], y[sl]
            s1t, s2t, s1k, s2k = s1[sl], s2[sl], "s1_%d" % sl, "s2_%d" % sl
            S.op("dve", lambda e: e.tensor_scalar(out=mean[:], in0=s1t[:], scalar1=1.0 / D, scalar2=None,
                                                  op0=ALU.mult), reads=[s1k], writes=["mean"])
            S.op("dve", lambda e: e.tensor_tensor(out=m2[:], in0=mean[:], in1=mean[:], op=ALU.mult),
                 reads=["mean"], writes=["m2"])
            S.op("dve", lambda e: e.scalar_tensor_tensor(out=var[:], in0=s2t[:], scalar=1.0 / D, in1=m2[:],
                                                         op0=ALU.mult, op1=ALU.subtract),
                 reads=[s2k, "m2"], writes=["var"])
            S.op("dve", lambda e: e.tensor_scalar(out=var[:], in0=var[:], scalar1=LN_EPS, scalar2=None,
                                                  op0=ALU.add), reads=["var"], writes=["var"])
            S.op("act", lambda e: e.activation(out=var[:], in_=var[:], func=AF.Sqrt),
                 reads=["var"], writes=["var"])
            S.op("dve", lambda e: e.reciprocal(out=rstd[:], in_=var[:]), reads=["var"], writes=["rstd"])
            S.op("dve", lambda e: e.scalar_tensor_tensor(out=cc[:], in0=mean[:], scalar=-1.0, in1=rstd[:],
                                                         op0=ALU.mult, op1=ALU.mult),
                 reads=["mean", "rstd"], writes=["cc"])
            S.op("dve", lambda e: e.tensor_tensor(out=zt[:], in0=zt[:], in1=bc_mid(rstd[:], KC), op=ALU.mult),
                 reads=[zk, "rstd"], writes=[zk])
            S.op("dve", lambda e: e.tensor_tensor(out=zt[:], in0=zt[:], in1=bc_mid(cc[:], KC), op=ALU.add),
                 reads=[zk, "cc"], writes=[zk])
            for kc in range(KC):
                S.op("act", lambda e, kc=kc: e.activation(
                    out=yt[:, kc, :], in_=zt[:, kc, :], func=AF.Identity,
                    bias=bcol[:, kc:kc + 1], scale=gcol[:, kc:kc + 1]),
                     reads=[zk, "gcol", "bcol"], writes=[yk], signal=(kc == KC - 1))
            store_y(blk, yt, yk, st)

        front(0)
        for blk in range(nblk):
            if blk + 1 < nblk:
                front(blk + 1)
            back(blk)
        S.end_phase()


W_OFF = dict(q=0, k=2048, v=4096, o=8192, g=12288, cq=12320, ckv=13344, kr=13856, ga=13920, gb=18016)
P1_CHUNKS = ([("q", i, 128) for i in range(16)] + [("k", i, 128) for i in range(16)] +
             [("cq", i, 128) for i in range(8)] + [("ckv", i, 128) for i in range(4)] +
             [("kr", i, 64) for i in range(2)] + [("v", i, 128) for i in range(32)] +
             [("o", i, 128) for i in range(32)] + [("ga", i, 128) for i in range(32)] +
             [("gb", i, 128) for i in range(32)])
P1_IDX = {(f, i): n for n, (f, i, m) in enumerate(P1_CHUNKS)}
NRES = TOWN + 2


def p1_pass(cx, T, pass_b):
    nc, S = cx.nc, cx.S
    tag = "p1b" if pass_b else "p1a"
    tok0 = (SEQ - NRES) if pass_b else 0
    doff = 2 if pass_b else 0
    otok0 = TOWN if pass_b else 0
    fams = ("k", "ckv", "kr", "v") if pass_b else ("q", "k", "cq", "ckv", "kr", "v", "o", "ga", "gb")
    chunks = [c for c in P1_CHUNKS if c[0] in fams]
    with ExitStack() as st:
        sb = lambda n, shp, dt: st.enter_context(nc.sbuf_tensor(tag + n, shp, dt))
        ps = lambda n, shp, dt: st.enter_context(nc.psum_tensor(tag + n, shp, dt))
        act = sb("act", [128, KC, NRES], BF16)
        NW = 3
        wb = [sb("w%d" % i, [128, KC, 128], BF16) for i in range(NW)]
        raw = sb("raw", [128, NRES + 2], F32)
        cv = sb("cv", [128, TOWN], F32)
        ob = [sb("ob%d" % i, [128, TOWN], BF16) for i in range(2)]
        vtk = [sb("vtk%d" % i, [128, 16, 128], BF16) for i in range(2)]
        ident = sb("ident", [128, 128], BF16)
        convw = sb("convw", [128, 32, 3], F32)
        wg = sb("wg", [128, KC, 32], BF16)
        bg = sb("bg", [128, 32], F32)
        gout = sb("gout", [128, 16, 32], F32)
        pm = [ps("pm%d" % i, [128, 512], F32) for i in range(4)]
        ph = ps("ph", [128, 512], F32)
        ptr = ps("ptr", [128, 16, 128], BF16)

        S.dma("sp", lambda e: e.dma_start(out=ident[:], in_=T["ident"]), writes=["ident"])
        S.dma("sp", lambda e: e.dma_start(out=convw[:], in_=T["convw"]), writes=["convw"])
        S.dma("sp", lambda e: e.dma_start(out=bg[:], in_=T["bg"]), writes=["bg"])
        S.dma("pool", lambda e: e.dma_start(out=wg[:], in_=T["wg"], max_dma_last_dim=4096), writes=["wg"])
        for kq in range(4):
            S.dma("sp" if kq % 2 == 0 else "act",
                  lambda e, kq=kq: e.dma_start(out=act[:, kq * 8:(kq + 1) * 8, :],
                                               in_=T["xnT"][:, kq * 8:(kq + 1) * 8, tok0:tok0 + NRES]),
                  writes=[("act", kq)])
        actk = [("act", kq) for kq in range(4)]
        S.op("dve", lambda e: e.memset(raw[:], 0.0), writes=["raw"])

        deferred = []

        def run_deferred():
            for f in deferred:
                f()
            deferred.clear()

        evac_flip = [0]

        def evac(out_ap, in_ap, reads, writes, func=None):
            if func is not None:
                S.op("act", lambda e: e.activation(out=out_ap, in_=in_ap, func=func), reads=reads, writes=writes)
                return
            evac_flip[0] ^= 1
            if evac_flip[0]:
                S.op("act", lambda e: e.activation(out=out_ap, in_=in_ap, func=AF.Copy), reads=reads, writes=writes)
            else:
                S.op("dve", lambda e: e.tensor_copy(out=out_ap, in_=in_ap), reads=reads, writes=writes)

        for ci, (fam, fi, M) in enumerate(chunks):
            slot = ci % NW
            wk = "w%d" % slot
            wt = wb[slot]
            gidx = P1_IDX[(fam, fi)]
            S.dma("pool", lambda e, wt=wt, gidx=gidx: e.dma_start(out=wt[:], in_=T["win_ch"][gidx],
                                                                  max_dma_last_dim=4096), writes=[wk])
            halo = fam in ("q", "k")
            boff = 0 if halo else doff
            for tb in range(4):
                for kc in range(KC):
                    S.op("pe", lambda e, wt=wt, kc=kc, tb=tb, boff=boff, M=M: e.matmul(
                        pm[tb][:M, :], lhsT=wt[:, kc, :M], rhs=act[:, kc, boff + tb * 512: boff + (tb + 1) * 512],
                        start=(kc == 0), stop=(kc == KC - 1)),
                         reads=[wk] + actk, writes=["pm%d" % tb], signal=(kc == KC - 1))
            if halo:
                for kc in range(KC):
                    S.op("pe", lambda e, wt=wt, kc=kc: e.matmul(
                        ph[:, 0:2], lhsT=wt[:, kc, :], rhs=act[:, kc, TOWN:TOWN + 2],
                        start=(kc == 0), stop=(kc == KC - 1)),
                         reads=[wk] + actk, writes=["ph"], signal=(kc == KC - 1))
            run_deferred()

            if halo:
                d0 = 0 if pass_b else 1
                c0 = 1 if pass_b else 0
                for tb in range(4):
                    evac(raw[:, d0 + tb * 512: d0 + (tb + 1) * 512], pm[tb][:, :], ["pm%d" % tb], ["raw"])
                evac(raw[:, d0 + TOWN: d0 + TOWN + 2], ph[:, 0:2], ["ph"], ["raw"])
                cw = (0 if fam == "q" else 16) + fi
                S.op("dve", lambda e, c0=c0, cw=cw: e.tensor_scalar(
                    out=cv[:], in0=raw[:, c0:c0 + TOWN], scalar1=convw[:, cw, 0:1], scalar2=None, op0=ALU.mult),
                     reads=["raw", "convw"], writes=["cv"])
                S.op("dve", lambda e, c0=c0, cw=cw: e.scalar_tensor_tensor(
                    out=cv[:], in0=raw[:, c0 + 1:c0 + 1 + TOWN], scalar=convw[:, cw, 1:2], in1=cv[:],
                    op0=ALU.mult, op1=ALU.add), reads=["raw", "convw", "cv"], writes=["cv"])
                S.op("dve", lambda e, c0=c0, cw=cw: e.scalar_tensor_tensor(
                    out=cv[:], in0=raw[:, c0 + 2:c0 + 2 + TOWN], scalar=convw[:, cw, 2:3], in1=cv[:],
                    op0=ALU.mult, op1=ALU.add), reads=["raw", "convw", "cv"], writes=["cv"])
                osl = ci % 2
                S.op("act", lambda e, osl=osl: e.activation(out=ob[osl][:], in_=cv[:], func=AF.Silu),
                     reads=["cv"], writes=["ob%d" % osl])
                dst = T["qcT"] if fam == "q" else T["kcT"]
                S.dma("sp", lambda e, osl=osl, dst=dst, fi=fi: e.dma_start(
                    out=dst[:, fi, otok0:otok0 + TOWN], in_=ob[osl][:]),
                      reads=["ob%d" % osl], writes=[(fam, fi, pass_b)])
            elif fam in ("cq", "ckv", "kr", "ga", "gb"):
                osl = ci % 2
                func = AF.Sigmoid if fam in ("ga", "gb") else None
                for tb in range(4):
                    evac(ob[osl][:M, tb * 512:(tb + 1) * 512], pm[tb][:M, :], ["pm%d" % tb], ["ob%d" % osl], func)
                dst = {"cq": "cqT", "ckv": "ckvT", "kr": "krT", "ga": "sgaT", "gb": "sgbT"}[fam]
                S.dma("sp", lambda e, osl=osl, dst=dst, fi=fi, M=M: e.dma_start(
                    out=T[dst][:M, fi, otok0:otok0 + TOWN], in_=ob[osl][:M, :]),
                      reads=["ob%d" % osl], writes=[(fam, fi, pass_b)])
            else:
                osl = ci % 2
                func = AF.Sigmoid if fam == "o" else None
                for tb in range(4):
                    evac(ob[osl][:, tb * 512:(tb + 1) * 512], pm[tb][:, :], ["pm%d" % tb], ["ob%d" % osl], func)

                def tr(osl=osl, fam=fam, fi=fi):
                    for j in range(16):
                        S.op("pe", lambda e, j=j: e.transpose(ptr[:, j, :], ob[osl][:, j * 128:(j + 1) * 128], ident[:]),
                             reads=["ob%d" % osl, "ident"], writes=["ptr"], signal=(j == 15))
                    S.op("dve", lambda e: e.tensor_copy(out=vtk[osl][:], in_=ptr[:]), reads=["ptr"],
                         writes=["vtk%d" % osl])
                    dst = T["vtok"] if fam == "v" else T["sotok"]
                    S.dma("act", lambda e: e.dma_start(
                        out=dst[otok0:otok0 + TOWN, fi * 128:(fi + 1) * 128].rearrange("(j p) c -> p j c", p=128),
                        in_=vtk[osl][:]), reads=["vtk%d" % osl], writes=[(fam, fi, pass_b)])
                deferred.append(tr)
        run_deferred()

        for tt in range(16):
            for kc in range(KC):
                S.op("pe", lambda e, tt=tt, kc=kc: e.matmul(
                    ph[:, 32:64], lhsT=act[:, kc, doff + tt * 128: doff + (tt + 1) * 128], rhs=wg[:, kc, :],
                    start=(kc == 0), stop=(kc == KC - 1)),
                     reads=["wg"] + actk, writes=["phg"], signal=(kc == KC - 1))
            S.op("dve", lambda e, tt=tt: e.tensor_tensor(out=gout[:, tt, :], in0=ph[:, 32:64], in1=bg[:], op=ALU.add),
                 reads=["phg", "bg"], writes=["gout"])
        S.dma("sp", lambda e: e.dma_start(
            out=T["gtok"][otok0:otok0 + TOWN, :].rearrange("(j p) c -> p j c", p=128), in_=gout[:]),
              reads=["gout"], writes=[("gtok", pass_b)])
        S.end_phase()


def mm_chunk(S, pm, pmk, wt, wk, act, actk, kcn, ntb, M=128, toff=0):
    for tb in range(ntb):
        for kc in range(kcn):
            S.op("pe", lambda e, tb=tb, kc=kc: e.matmul(
                pm[tb][:M, :], lhsT=wt[:, kc, :M], rhs=act[:, kc, toff + tb * 512: toff + (tb + 1) * 512],
                start=(kc == 0), stop=(kc == kcn - 1)),
                 reads=[wk] + actk, writes=[pmk[tb]], signal=(kc == kcn - 1))


def load_resident(S, act, src, kcn, key):
    n = 4 if kcn >= 4 else 1
    step = kcn // n
    keys = []
    for i in range(n):
        S.dma("sp" if i % 2 == 0 else "act",
              lambda e, i=i: e.dma_start(out=act[:, i * step:(i + 1) * step, :], in_=src[:, i * step:(i + 1) * step, :]),
              writes=[(key, i)])
        keys.append((key, i))
    return keys


def p4_phase(cx, T, which):
    nc, S = cx.nc, cx.S
    tag = "p4" + which
    src = {"a": "AT", "b": "BT", "c": "mixT"}[which]
    wname = {"a": "wpm_ch", "b": "wpa_ch", "c": "wout_ch"}[which]
    with ExitStack() as st:
        sb = lambda n, shp, dt: st.enter_context(nc.sbuf_tensor(tag + n, shp, dt))
        ps = lambda n, shp, dt: st.enter_context(nc.psum_tensor(tag + n, shp, dt))
        act = sb("act", [128, KC, TOWN], BF16)
        NW = 3
        wb = [sb("w%d" % i, [128, KC, 128], BF16) for i in range(NW)]
        e1 = [sb("e1_%d" % i, [128, 512], F32) for i in range(3)]
        e2 = [sb("e2_%d" % i, [128, 512], F32 if which == "c" else BF16) for i in range(3)]
        o1 = [sb("o1_%d" % i, [128, 512], F32) for i in range(3)]
        o2 = [sb("o2_%d" % i, [128, 512], BF16) for i in range(3)]
        pm = [ps("pm%d" % i, [128, 512], F32) for i in range(8)]
        actk = load_resident(S, act, T[src], KC, "act")
        n = 0
        for ci in range(KC):
            slot = ci % NW
            wt, wk = wb[slot], "w%d" % slot
            S.dma("pool", lambda e, wt=wt, ci=ci: e.dma_start(out=wt[:], in_=T[wname][ci], max_dma_last_dim=4096),
                  writes=[wk])
            pset = [pm[(ci % 2) * 4 + i] for i in range(4)]
            pkey = ["pm%d" % ((ci % 2) * 4 + i) for i in range(4)]
            mm_chunk(S, pset, pkey, wt, wk, act, actk, KC, 4)
            for tb in range(4):
                b = n % 3
                n += 1
                tsl = slice(tb * 512, (tb + 1) * 512)
                if which == "a":
                    S.dma("sp", lambda e, b=b, ci=ci, tsl=tsl: e.dma_start(out=e2[b][:], in_=T["sgaT"][:, ci, tsl]),
                          writes=["e2_%d" % b])
                    S.op("dve", lambda e, b=b, tb=tb, pset=pset: e.tensor_tensor(out=o1[b][:], in0=pset[tb][:], in1=e2[b][:], op=ALU.mult),
                         reads=[pkey[tb], "e2_%d" % b], writes=["o1_%d" % b])
                    S.dma("act", lambda e, b=b, ci=ci, tsl=tsl: e.dma_start(out=T["mixaT"][:, ci, tsl], in_=o1[b][:]),
                          reads=["o1_%d" % b], writes=[("mixaT", ci, tb)])
                elif which == "b":
                    S.dma("sp", lambda e, b=b, ci=ci, tsl=tsl: e.dma_start(out=e2[b][:], in_=T["sgbT"][:, ci, tsl]),
                          writes=["e2_%d" % b])
                    S.dma("sp", lambda e, b=b, ci=ci, tsl=tsl: e.dma_start(out=e1[b][:], in_=T["mixaT"][:, ci, tsl]),
                          writes=["e1_%d" % b])
                    S.op("dve", lambda e, b=b, tb=tb, pset=pset: e.tensor_tensor(out=o1[b][:], in0=pset[tb][:], in1=e2[b][:], op=ALU.mult),
                         reads=[pkey[tb], "e2_%d" % b], writes=["o1_%d" % b])
                    S.op("pool", lambda e, b=b: e.tensor_tensor(out=o2[b][:], in0=o1[b][:], in1=e1[b][:], op=ALU.add),
                         reads=["o1_%d" % b, "e1_%d" % b], writes=["o2_%d" % b])
                    S.dma("act", lambda e, b=b, ci=ci, tsl=tsl: e.dma_start(out=T["mixT"][:, ci, tsl], in_=o2[b][:]),
                          reads=["o2_%d" % b], writes=[("mixT", ci, tb)])
                else:
                    S.dma("sp", lambda e, b=b, ci=ci, tsl=tsl: e.dma_start(out=e2[b][:], in_=T["x0T"][:, ci, tsl]),
                          writes=["e2_%d" % b])
                    S.op("dve", lambda e, b=b, tb=tb, pset=pset: e.scalar_tensor_tensor(
                        out=o1[b][:], in0=e2[b][:], scalar=ALPHA, in1=pset[tb][:], op0=ALU.mult, op1=ALU.add),
                         reads=[pkey[tb], "e2_%d" % b], writes=["o1_%d" % b])
                    S.dma("act", lambda e, b=b, ci=ci, tsl=tsl: e.dma_start(out=T["z1T"][:, ci, tsl], in_=o1[b][:]),
                          reads=["o1_%d" % b], writes=[("z1T", ci, tb)])
        S.end_phase()


def router_phase(cx, T):
    nc, S = cx.nc, cx.S
    tag = "rt"
    BIG = 1.0e4
    with ExitStack() as st:
        sb = lambda n, shp, dt: st.enter_context(nc.sbuf_tensor(tag + n, shp, dt))
        ps = lambda n, shp, dt: st.enter_context(nc.psum_tensor(tag + n, shp, dt))
        wr = sb("wr", [128, KC, 40], F32)
        br = sb("br", [128, 40], F32)
        identf = sb("identf", [128, 128], F32)
        xf = [sb("xf%d" % i, [128, KC, 128], F32) for i in range(2)]
        L = sb("L", [128, 40], F32)
        em = sb("em", [128, 32], F32)
        em2 = sb("em2", [128, 32], F32)
        m1 = sb("m1", [128, 32], F32)
        m2 = sb("m2", [128, 32], F32)
        comb = sb("comb", [128, 32], F32)
        gmask = sb("gmask", [128, 8], F32)
        ex = sb("ex", [128, 8], F32)
        sc = sb("sc", [128, 16], F32)
        combT = sb("combT", [32, TOWN], F32)
        pl = ps("pl", [128, 512], F32)
        pt = ps("pt", [128, 512], F32)
        S.dma("sp", lambda e: e.dma_start(out=wr[:], in_=T["wr"]), writes=["wr"])
        S.dma("sp", lambda e: e.dma_start(out=br[:], in_=T["br"]), writes=["br"])
        S.dma("sp", lambda e: e.dma_start(out=identf[:], in_=T["identf"]), writes=["identf"])
        c = lambda i: sc[:, i:i + 1]
        for tt in range(TOWN // 128):
            sl = tt % 2
            xk = "xf%d" % sl
            S.dma("sp" if sl == 0 else "act",
                  lambda e, sl=sl, tt=tt: e.dma_start(out=xf[sl][:], in_=T["x1T"][:, :, tt * 128:(tt + 1) * 128]),
                  writes=[xk])
            for kc in range(KC):
                S.op("pe", lambda e, sl=sl, kc=kc: e.matmul(pl[:, 0:40], lhsT=xf[sl][:, kc, :], rhs=wr[:, kc, :],
                                                            start=(kc == 0), stop=(kc == KC - 1)),
                     reads=[xk, "wr"], writes=["pl"], signal=(kc == KC - 1))
            dv = lambda fn, r, w: S.op("dve", fn, reads=r, writes=w)
            dv(lambda e: e.tensor_tensor(out=L[:], in0=pl[:, 0:40], in1=br[:], op=ALU.add), ["pl", "br"], ["L"])
            dv(lambda e: e.tensor_reduce(out=c(0), in_=L[:, 0:8], axis=mybir.AxisListType.X, op=ALU.max), ["L"], ["sc"])
            dv(lambda e: e.tensor_scalar(out=gmask[:], in0=L[:, 0:8], scalar1=c(0), scalar2=None, op0=ALU.is_equal),
               ["L", "sc"], ["gmask"])
            dv(lambda e: e.tensor_scalar(out=c(1), in0=c(0), scalar1=-1.0, scalar2=None, op0=ALU.mult), ["sc"], ["sc"])
            S.op("act", lambda e: e.activation(out=ex[:], in_=L[:, 0:8], func=AF.Exp, bias=c(1), scale=1.0,
                                               accum_out=c(2)), reads=["L", "sc"], writes=["ex", "sc"])
            dv(lambda e: e.reciprocal(out=c(3), in_=c(2)), ["sc"], ["sc"])
            dv(lambda e: e.tensor_scalar(out=gmask[:], in0=gmask[:], scalar1=BIG, scalar2=-BIG, op0=ALU.mult, op1=ALU.add),
               ["gmask"], ["gmask"])
            dv(lambda e: e.tensor_tensor(out=em[:].rearrange("p (g x) -> p g x", x=4),
                                         in0=L[:, 8:40].rearrange("p (g x) -> p g x", x=4),
                                         in1=gmask[:].unsqueeze(2).to_broadcast([128, 8, 4]), op=ALU.add),
               ["L", "gmask"], ["em"])
            dv(lambda e: e.tensor_reduce(out=c(4), in_=em[:], axis=mybir.AxisListType.X, op=ALU.max), ["em"], ["sc"])
            dv(lambda e: e.tensor_scalar(out=m1[:], in0=em[:], scalar1=c(4), scalar2=None, op0=ALU.is_equal),
               ["em", "sc"], ["m1"])
            dv(lambda e: e.scalar_tensor_tensor(out=em2[:], in0=m1[:], scalar=-BIG, in1=em[:], op0=ALU.mult, op1=ALU.add),
               ["m1", "em"], ["em2"])
            dv(lambda e: e.tensor_reduce(out=c(5), in_=em2[:], axis=mybir.AxisListType.X, op=ALU.max), ["em2"], ["sc"])
            dv(lambda e: e.tensor_scalar(out=m2[:], in0=em2[:], scalar1=c(5), scalar2=None, op0=ALU.is_equal),
               ["em2", "sc"], ["m2"])
            dv(lambda e: e.tensor_tensor(out=c(6), in0=c(5), in1=c(4), op=ALU.subtract), ["sc"], ["sc"])
            S.op("act", lambda e: e.activation(out=c(7), in_=c(6), func=AF.Exp), reads=["sc"], writes=["sc"])
            dv(lambda e: e.tensor_scalar(out=c(8), in0=c(7), scalar1=1.0, scalar2=None, op0=ALU.add), ["sc"], ["sc"])
            dv(lambda e: e.reciprocal(out=c(8), in_=c(8)), ["sc"], ["sc"])
            dv(lambda e: e.tensor_tensor(out=c(9), in0=c(7), in1=c(8), op=ALU.mult), ["sc"], ["sc"])
            dv(lambda e: e.tensor_tensor(out=c(10), in0=c(8), in1=c(3), op=ALU.mult), ["sc"], ["sc"])
            dv(lambda e: e.tensor_tensor(out=c(11), in0=c(9), in1=c(3), op=ALU.mult), ["sc"], ["sc"])
            dv(lambda e: e.tensor_scalar(out=comb[:], in0=m1[:], scalar1=c(10), scalar2=None, op0=ALU.mult),
               ["m1", "sc"], ["comb"])
            dv(lambda e: e.scalar_tensor_tensor(out=comb[:], in0=m2[:], scalar=c(11), in1=comb[:], op0=ALU.mult, op1=ALU.add),
               ["m2", "sc", "comb"], ["comb"])
            S.op("pe", lambda e: e.transpose(pt[0:32, 0:128], comb[:], identf[:]), reads=["comb", "identf"], writes=["pt"])
            S.op("act", lambda e, tt=tt: e.activation(out=combT[:, tt * 128:(tt + 1) * 128], in_=pt[0:32, 0:128], func=AF.Copy),
                 reads=["pt"], writes=["combT"])
        S.dma("sp", lambda e: e.dma_start(out=T["combT"], in_=combT[:]), reads=["combT"], writes=["combT_d"])
        S.end_phase()


def moe_phase(cx, T, n_experts=32):
    nc, S = cx.nc, cx.S
    tag = "moe"
    TS = 1024
    with ExitStack() as st:
        sb = lambda n, shp, dt: st.enter_context(nc.sbuf_tensor(tag + n, shp, dt))
        ps = lambda n, shp, dt: st.enter_context(nc.psum_tensor(tag + n, shp, dt))
        act = sb("act", [128, KC, TS], BF16)
        hT = [sb("hT%d" % i, [128, 8, TS], BF16) for i in range(2)]
        NW = 4
        wb = [sb("w%d" % i, [128, KC, 128], BF16) for i in range(NW)]
        wd = [sb("wd%d" % i, [128, 8, 128], BF16) for i in range(NW)]
        sg = [sb("sg%d" % i, [128, TS], BF16) for i in range(2)]
        sgc = [sb("sgc%d" % i, [128, TS], F32) for i in range(2)]
        cb = [sb("cb%d" % i, [128, TS], F32) for i in range(2)]
        cmask = sb("cmask", [32, TS], F32)
        combT = sb("combT", [32, TOWN], F32)
        ident32 = sb("ident32", [32, 32], F32)
        ones32 = sb("ones32", [32, 128], F32)
        ai = [sb("ai%d" % i, [128, TS], F32) for i in range(2)]
        ao = [sb("ao%d" % i, [128, TS], F32) for i in range(2)]
        pm = [ps("pm%d" % i, [128, 512], F32) for i in range(8)]
        S.dma("sp", lambda e: e.dma_start(out=combT[:], in_=T["combT"]), writes=["combT"])
        S.dma("sp", lambda e: e.dma_start(out=ident32[:], in_=T["identf"][0:32, 0:32]), writes=["ident32"])
        S.op("dve", lambda e: e.memset(ones32[:], 1.0), writes=["ones32"])
        wcnt = [0, 0]
        acnt = [0]

        def gate_up(s, e_, hsl):
            tsl = slice(s * TS, (s + 1) * TS)
            csl = e_ % 2
            S.op("dve", lambda e: e.tensor_scalar(out=cmask[:], in0=combT[:, tsl], scalar1=ident32[:, e_:e_ + 1],
                                                  scalar2=None, op0=ALU.mult),
                 reads=["combT", "ident32"], writes=["cmask"])
            for tb in range(2):
                S.op("pe", lambda e, tb=tb: e.matmul(pm[6 + tb][:, :], lhsT=ones32[:], rhs=cmask[:, tb * 512:(tb + 1) * 512],
                                                     start=True, stop=True),
                     reads=["cmask", "ones32"], writes=["pm%d" % (6 + tb)])
                S.op("act", lambda e, tb=tb: e.activation(out=cb[csl][:, tb * 512:(tb + 1) * 512], in_=pm[6 + tb][:, :], func=AF.Copy),
                     reads=["pm%d" % (6 + tb)], writes=["cb%d" % csl])
            for j in range(8):
                for which in range(2):
                    slot = wcnt[0] % NW
                    wcnt[0] += 1
                    wt, wk = wb[slot], "w%d" % slot
                    src = T["wge_ch"] if which == 0 else T["wue_ch"]
                    S.dma("pool", lambda e, wt=wt, src=src, j=j: e.dma_start(out=wt[:], in_=src[e_, j], max_dma_last_dim=4096),
                          writes=[wk])
                    pset = [pm[which * 2 + i] for i in range(2)]
                    pkey = ["pm%d" % (which * 2 + i) for i in range(2)]
                    mm_chunk(S, pset, pkey, wt, wk, act, actk, KC, 2)
                    gsl = j % 2
                    if which == 0:
                        for tb in range(2):
                            S.op("act", lambda e, tb=tb, pset=pset, gsl=gsl: e.activation(
                                out=sg[gsl][:, tb * 512:(tb + 1) * 512], in_=pset[tb][:, :], func=AF.Silu),
                                 reads=[pkey[tb]], writes=["sg%d" % gsl], signal=(tb == 1))
                        S.op("dve", lambda e, gsl=gsl: e.tensor_tensor(out=sgc[gsl][:], in0=sg[gsl][:], in1=cb[csl][:], op=ALU.mult),
                             reads=["sg%d" % gsl, "cb%d" % csl], writes=["sgc%d" % gsl])
                    else:
                        for tb in range(2):
                            S.op("dve", lambda e, tb=tb, pset=pset, gsl=gsl, j=j: e.tensor_tensor(
                                out=hT[hsl][:, j, tb * 512:(tb + 1) * 512], in0=pset[tb][:, :],
                                in1=sgc[gsl][:, tb * 512:(tb + 1) * 512], op=ALU.mult),
                                 reads=[pkey[tb], "sgc%d" % gsl], writes=["hT%d" % hsl])

        def down(s, e_, hsl, first):
            for c in range(KC):
                slot = wcnt[1] % NW
                wcnt[1] += 1
                wt, wk = wd[slot], "wd%d" % slot
                S.dma("pool", lambda e, wt=wt, c=c: e.dma_start(out=wt[:], in_=T["wde_ch"][e_, c], max_dma_last_dim=4096),
                      writes=[wk])
                pset = [pm[4 + i] for i in range(2)]
                pkey = ["pm%d" % (4 + i) for i in range(2)]
                mm_chunk(S, pset, pkey, wt, wk, hT[hsl], ["hT%d" % hsl], 8, 2)
                b = acnt[0] % 2
                acnt[0] += 1
                dst = T["moeT"][:, c, s * TS:(s + 1) * TS]
                if first:
                    for tb in range(2):
                        S.op("dve", lambda e, tb=tb, b=b: e.tensor_copy(out=ao[b][:, tb * 512:(tb + 1) * 512], in_=pset[tb][:, :]),
                             reads=[pkey[tb]], writes=["ao%d" % b], signal=(tb == 1))
                else:
                    S.dma("act", lambda e, b=b, dst=dst: e.dma_start(out=ai[b][:], in_=dst), reads=[("moeT", c, s)], writes=["ai%d" % b])
                    for tb in range(2):
                        S.op("dve", lambda e, tb=tb, b=b: e.tensor_tensor(
                            out=ao[b][:, tb * 512:(tb + 1) * 512], in0=pset[tb][:, :], in1=ai[b][:, tb * 512:(tb + 1) * 512], op=ALU.add),
                             reads=[pkey[tb], "ai%d" % b], writes=["ao%d" % b], signal=(tb == 1))
                S.dma("sp", lambda e, b=b, dst=dst: e.dma_start(out=dst, in_=ao[b][:]), reads=["ao%d" % b], writes=[("moeT", c, s)])

        for s in range(TOWN // TS):
            actk = load_resident(S, act, T["x1bT"][:, :, s * TS:(s + 1) * TS], KC, "act")
            gate_up(s, 0, 0)
            for e_ in range(n_experts):
                if e_ + 1 < n_experts:
                    gate_up(s, e_ + 1, (e_ + 1) % 2)
                down(s, e_, e_ % 2, e_ == 0)
        S.end_phase()


ATTN_SCALE = 192.0 ** -0.5


def mla_phase(cx, T, n_heads=32):
    nc, S = cx.nc, cx.S
    tag = "mla"
    with ExitStack() as st:
        sb = lambda n, shp, dt: st.enter_context(nc.sbuf_tensor(tag + n, shp, dt))
        ps = lambda n, shp, dt: st.enter_context(nc.psum_tensor(tag + n, shp, dt))
        cqn = sb("cqn", [128, 8, TOWN], BF16)
        ckvn = sb("ckvn", [128, 4, SEQ], BF16)
        krr = sb("krr", [64, 2, SEQ], BF16)
        cosT = sb("cos", [64, SEQ], F32)
        sinT = sb("sin", [64, SEQ], F32)
        krope = sb("krope", [64, SEQ], BF16)
        ones = sb("ones", [128, 128], BF16)
        ident = sb("ident", [128, 128], BF16)
        qng = sb("qng", [128, 8], F32)
        kvng = sb("kvng", [128, 4], F32)
        sq = [sb("sq%d" % i, [128, 512], BF16) for i in range(2)]
        rb = sb("rb", [128, 512], F32)
        t1 = sb("t1", [64, 1024], F32)
        t2 = sb("t2", [64, 1024], F32)
        kT = sb("kT", [128, SEQ], BF16)
        vaug = sb("vaug", [128, 32, 129], BF16)
        qn = sb("qn", [128, TOWN], BF16)
        qr = sb("qr", [64, TOWN], BF16)
        wq = [sb("wq%d" % i, [128, 8, 256], BF16) for i in range(2)]
        wkv = [sb("wkv%d" % i, [128, 4, 256], BF16) for i in range(2)]
        PT = [sb("PT%d" % i, [128, 512], BF16) for i in range(2)]
        osb = [sb("osb%d" % i, [128, 128], BF16) for i in range(2)]
        rinv = sb("rinv", [128, 512], F32)
        BTh = [sb("BTh%d" % i, [128, TOWN], BF16) for i in range(2)]
        pss = [ps("pss%d" % i, [128, 512], F32) for i in range(2)]
        po = [ps("po%d" % i, [128, 512], F32) for i in range(4)]
        pj = ps("pj", [128, 512], F32)
        pq = ps("pq", [128, 512], F32)
        pqb = pq[:].bitcast(BF16)

        S.dma("sp", lambda e: e.dma_start(out=cqn[:], in_=T["cqT"]), writes=["cqn"])
        S.dma("act", lambda e: e.dma_start(out=ckvn[:], in_=T["ckvT"]), writes=["ckvn"])
        S.dma("sp", lambda e: e.dma_start(out=krr[:], in_=T["krT"]), writes=["krr"])
        S.dma("act", lambda e: e.dma_start(out=cosT[:], in_=T["cosT"]), writes=["cos"])
        S.dma("sp", lambda e: e.dma_start(out=sinT[:], in_=T["sinT"]), writes=["sin"])
        S.dma("sp", lambda e: e.dma_start(out=ident[:], in_=T["ident"]), writes=["ident"])
        S.dma("sp", lambda e: e.dma_start(out=qng[:], in_=T["qng"]), writes=["qng"])
        S.dma("sp", lambda e: e.dma_start(out=kvng[:], in_=T["kvng"]), writes=["kvng"])
        S.op("dve", lambda e: e.memset(ones[:], 1.0), writes=["ones"])
        S.op("dve", lambda e: e.memset(vaug[:, :, 128:129], 1.0), writes=["vaug"])

        def rms(buf, bk, nkc, ntb, gcol, gk, dlat):
            n = 0
            for tb in range(ntb):
                tsl = slice(tb * 512, (tb + 1) * 512)
                for kc in range(nkc):
                    b = n % 2
                    n += 1
                    S.op("pool", lambda e, b=b, kc=kc, tsl=tsl: e.tensor_tensor(out=sq[b][:], in0=buf[:, kc, tsl], in1=buf[:, kc, tsl], op=ALU.mult),
                         reads=[bk], writes=["sq%d" % b])
                    S.op("pe", lambda e, b=b, kc=kc: e.matmul(pj[:, :], lhsT=ones[:], rhs=sq[b][:], start=(kc == 0), stop=(kc == nkc - 1)),
                         reads=["sq%d" % b, "ones"], writes=["pj"])
                S.op("dve", lambda e: e.tensor_scalar(out=rb[:], in0=pj[:, :], scalar1=1.0 / dlat, scalar2=RMS_EPS, op0=ALU.mult, op1=ALU.add),
                     reads=["pj"], writes=["rb"])
                S.op("act", lambda e: e.activation(out=rb[:], in_=rb[:], func=AF.Sqrt), reads=["rb"], writes=["rb"])
                S.op("dve", lambda e: e.reciprocal(out=rb[:], in_=rb[:]), reads=["rb"], writes=["rb"])
                for kc in range(nkc):
                    S.op("dve", lambda e, kc=kc, tsl=tsl: e.scalar_tensor_tensor(
                        out=buf[:, kc, tsl], in0=buf[:, kc, tsl], scalar=gcol[:, kc:kc + 1], in1=rb[:], op0=ALU.mult, op1=ALU.mult),
                         reads=[bk, gk, "rb"], writes=[bk])

        rms(cqn, "cqn", 8, 4, qng, "qng", 1024.0)
        rms(ckvn, "ckvn", 4, 8, kvng, "kvng", 512.0)
        for blk in range(4):
            tsl = slice(blk * 1024, (blk + 1) * 1024)
            S.op("dve", lambda e, tsl=tsl: e.tensor_tensor(out=t1[:], in0=krr[:, 0, tsl], in1=cosT[:, tsl], op=ALU.mult),
                 reads=["krr", "cos"], writes=["t1"])
            S.op("pool", lambda e, tsl=tsl: e.tensor_tensor(out=t2[:], in0=krr[:, 1, tsl], in1=sinT[:, tsl], op=ALU.mult),
                 reads=["krr", "sin"], writes=["t2"])
            S.op("dve", lambda e, tsl=tsl: e.tensor_tensor(out=krope[:, tsl], in0=t1[:], in1=t2[:], op=ALU.add),
                 reads=["t1", "t2"], writes=["krope"])

        flip = [0]

        def evac(out_ap, in_ap, reads, writes):
            flip[0] ^= 1
            if flip[0]:
                S.op("act", lambda e: e.activation(out=out_ap, in_=in_ap, func=AF.Copy), reads=reads, writes=writes)
            else:
                S.op("dve", lambda e: e.tensor_copy(out=out_ap, in_=in_ap), reads=reads, writes=writes)

        for h in range(n_heads):
            ws = h % 2
            wqt, wkvt = wq[ws], wkv[ws]
            wqk, wkvk = "wq%d" % ws, "wkv%d" % ws
            S.dma("pool", lambda e, wqt=wqt, h=h: e.dma_start(out=wqt[:], in_=T["wuq_h"][h], max_dma_last_dim=4096), writes=[wqk])
            S.dma("pool", lambda e, wkvt=wkvt, h=h: e.dma_start(out=wkvt[:], in_=T["wukv_h"][h], max_dma_last_dim=4096), writes=[wkvk])
            for tb in range(8):
                tsl = slice(tb * 512, (tb + 1) * 512)
                pb, pbk = (pj, "pj") if tb % 2 == 0 else (pq, "pq")
                for kc in range(4):
                    S.op("pe", lambda e, kc=kc, tsl=tsl, pb=pb, wkvt=wkvt: e.matmul(pb[:, :], lhsT=wkvt[:, kc, 0:128], rhs=ckvn[:, kc, tsl],
                                                                    start=(kc == 0), stop=(kc == 3)),
                         reads=[wkvk, "ckvn"], writes=[pbk], signal=(kc == 3))
                evac(kT[:, tsl], pb[:, :], [pbk], ["kT"])
            for g in range(8):
                pb, pbk = (pj, "pj") if g % 2 == 0 else (pq, "pq")
                for j in range(4):
                    tt = g * 4 + j
                    for kc in range(4):
                        S.op("pe", lambda e, kc=kc, tt=tt, j=j, pb=pb, wkvt=wkvt: e.matmul(
                            pb[:, j * 128:(j + 1) * 128], lhsT=ckvn[:, kc, tt * 128:(tt + 1) * 128], rhs=wkvt[:, kc, 128:256],
                            start=(kc == 0), stop=(kc == 3)),
                             reads=[wkvk, "ckvn"], writes=[pbk], signal=(kc == 3 and j == 3))
                evac(vaug[:, g * 4:(g + 1) * 4, 0:128], pb[:, :].rearrange("p (j c) -> p j c", c=128), [pbk], ["vaug"])
            for tb in range(4):
                tsl = slice(tb * 512, (tb + 1) * 512)
                for kc in range(8):
                    S.op("pe", lambda e, kc=kc, tsl=tsl, wqt=wqt: e.matmul(pj[:, :], lhsT=wqt[:, kc, 0:128], rhs=cqn[:, kc, tsl],
                                                                  start=(kc == 0), stop=(kc == 7)),
                         reads=[wqk, "cqn"], writes=["pj"], signal=(kc == 7))
                evac(qn[:, tsl], pj[:, :], ["pj"], ["qn"])
                for kc in range(8):
                    S.op("pe", lambda e, kc=kc, tsl=tsl, wqt=wqt: e.matmul(pj[0:64, :], lhsT=wqt[:, kc, 128:192], rhs=cqn[:, kc, tsl],
                                                                  start=(kc == 0), stop=(kc == 7)),
                         reads=[wqk, "cqn"], writes=["pj"], signal=(kc == 7))
                for kc in range(8):
                    S.op("pe", lambda e, kc=kc, tsl=tsl, wqt=wqt: e.matmul(pq[0:64, :], lhsT=wqt[:, kc, 192:256], rhs=cqn[:, kc, tsl],
                                                                  start=(kc == 0), stop=(kc == 7)),
                         reads=[wqk, "cqn"], writes=["pq"], signal=(kc == 7))
                S.op("dve", lambda e, tsl=tsl: e.tensor_tensor(out=t1[:, 0:512], in0=pj[0:64, :], in1=cosT[:, tsl], op=ALU.mult),
                     reads=["pj", "cos"], writes=["t1"])
                S.op("dve", lambda e, tsl=tsl: e.tensor_tensor(out=t2[:, 0:512], in0=pq[0:64, :], in1=sinT[:, tsl], op=ALU.mult),
                     reads=["pq", "sin"], writes=["t2"])
                S.op("pool", lambda e, tsl=tsl: e.tensor_tensor(out=qr[:, tsl], in0=t1[:, 0:512], in1=t2[:, 0:512], op=ALU.add),
                     reads=["t1", "t2"], writes=["qr"])
            bsl = h % 2
            for qb in range(4):
                qsl = slice(qb * 512, (qb + 1) * 512)
                pa, pak = po[(qb % 2) * 2], "po%d" % ((qb % 2) * 2)
                pbb, pbbk = po[(qb % 2) * 2 + 1], "po%d" % ((qb % 2) * 2 + 1)

                def score(kt, qsl=qsl):
                    ksl = slice(kt * 128, (kt + 1) * 128)
                    pb, pbk = pss[kt % 2], "pss%d" % (kt % 2)
                    S.op("pe", lambda e: e.matmul(pb[:, :], lhsT=kT[:, ksl], rhs=qn[:, qsl], start=True, stop=False),
                         reads=["kT", "qn"], writes=[pbk], signal=False)
                    S.op("pe", lambda e: e.matmul(pb[:, :], lhsT=krope[:, ksl], rhs=qr[:, qsl], start=False, stop=True),
                         reads=["krope", "qr"], writes=[pbk])

                score(0)
                for kt in range(32):
                    if kt + 1 < 32:
                        score(kt + 1)
                    pb, pbk = pss[kt % 2], "pss%d" % (kt % 2)
                    pt, ptk = PT[kt % 2], "PT%d" % (kt % 2)
                    S.op("act", lambda e, pb=pb, pt=pt: e.activation(out=pt[:], in_=pb[:, :], func=AF.Exp, scale=ATTN_SCALE),
                         reads=[pbk], writes=[ptk])
                    S.op("pe", lambda e, pt=pt, kt=kt, pa=pa: e.matmul(pa[:, :], lhsT=vaug[:, kt, 0:128], rhs=pt[:],
                                                                start=(kt == 0), stop=(kt == 31)),
                         reads=[ptk, "vaug"], writes=[pak], signal=False)
                    S.op("pe", lambda e, pt=pt, kt=kt, pbb=pbb: e.matmul(pbb[:, :], lhsT=ones[:], rhs=pt[:],
                                                                  start=(kt == 0), stop=(kt == 31)),
                         reads=[ptk, "ones"], writes=[pbbk])
                S.op("dve", lambda e, pbb=pbb: e.reciprocal(out=rinv[:], in_=pbb[:, :]), reads=[pbbk], writes=["rinv"])
                S.op("dve", lambda e, pa=pa, qsl=qsl, bsl=bsl: e.tensor_tensor(out=BTh[bsl][:, qsl], in0=pa[:, :], in1=rinv[:], op=ALU.mult),
                     reads=[pak, "rinv"], writes=["BTh%d" % bsl])
            S.dma("sp", lambda e, h=h, bsl=bsl: e.dma_start(out=T["BT"][:, h, :], in_=BTh[bsl][:]),
                  reads=["BTh%d" % bsl], writes=[("BT", h)])
        S.end_phase()


def mlstm_phase(cx, T, n_own=32, n_all=64):
    nc, S = cx.nc, cx.S
    tag = "ml"
    NCH = n_all
    with ExitStack() as st:
        sb = lambda n, shp, dt: st.enter_context(nc.sbuf_tensor(tag + n, shp, dt))
        ps = lambda n, shp, dt: st.enter_context(nc.psum_tensor(tag + n, shp, dt))
        G = sb("G", [64, NCH, 32], F32)
        tri = sb("tri", [64, 4, 64], F32)
        maskb = sb("maskb", [64, 2, 64], F32)
        ones64 = sb("ones64", [64, 128], F32)
        ones64b = sb("ones64b", [64, 2], BF16)
        ident = sb("ident", [128, 128], BF16)
        hng = sb("hng", [128, 32], F32)
        lf = sb("lf", [64, NCH * 8], F32)
        tmpg = sb("tmpg", [64, NCH * 8], F32)
        u = [sb("u%d" % d, [64, NCH * 8], F32) for d in range(2)]
        fl = [sb("fl%d" % d, [64, NCH * 8], F32) for d in range(2)]
        wsx = [sb("ws%d" % d, [64, NCH * 8], F32) for d in range(2)]
        dec = [sb("dec%d" % d, [128, NCH * 8], F32) for d in range(2)]
        C = sb("C", [128, 8, 2, 512], F32)
        Cb = sb("Cb", [128, 8, 2, 512], BF16)
        nst = sb("nst", [128, 8, 2], F32)
        nb = sb("nb", [128, 8, 2], BF16)
        qTc = [sb("qTc%d" % i, [128, 16, 64], BF16) for i in range(2)]
        kTc = [sb("kTc%d" % i, [128, 16, 64], BF16) for i in range(2)]
        vch = [sb("vch%d" % i, [64, 4096], BF16) for i in range(2)]
        soch = sb("soch", [64, 4096], BF16)
        hfin = sb("hfin", [64, 4096], F32)
        hsum = sb("hsum", [64, 4096], F32)
        kws = [sb("kws%d" % i, [64, 8, 256], BF16) for i in range(2)]
        stf = sb("stf", [64, 8, 64], F32)
        sTm = [sb("sTm%d" % i, [64, 8, 64], BF16) for i in range(2)]
        rden = sb("rden", [64, 8], F32)
        ss = sb("ss", [64, 8], F32)
        sqj = sb("sqj", [64, 512], F32)
        Abf = sb("Abf", [64, 4096], BF16)
        ATs = sb("ATs", [128, 32, 256], BF16)
        pst = ps("pst", [128, 512], F32)
        ptr_ = ps("ptr", [128, 1024], F32)
        ptrb = ptr_[:].bitcast(BF16)
        pn = [ps("pn%d" % i, [128, 512], F32) for i in range(2)]
        pu = [ps("pu%d" % i, [128, 512], F32) for i in range(2)]
        pd = ps("pd", [128, 512], F32)

        S.dma("sp", lambda e: e.dma_start(out=G[:], in_=T["gtok"].rearrange("(c p) g -> p c g", p=64)), writes=["G"])
        S.dma("sp", lambda e: e.dma_start(out=tri[:], in_=T["tri"]), writes=["tri"])
        S.dma("sp", lambda e: e.dma_start(out=ident[:], in_=T["ident"]), writes=["ident"])
        S.dma("sp", lambda e: e.dma_start(out=hng[:], in_=T["hng"]), writes=["hng"])
        S.op("dve", lambda e: e.memset(ones64[:], 1.0), writes=["ones64"])
        S.op("dve", lambda e: e.memset(ones64b[:], 1.0), writes=["ones64b"])
        S.op("dve", lambda e: e.tensor_copy(out=maskb[:], in_=tri[:, 0:2, :]), reads=["tri"], writes=["maskb"])

        NG = NCH * 8
        v3 = lambda t: t[:].rearrange("p (c h) -> p c h", h=8)
        for d in range(2):
            iv = G[:, :, 16 * d:16 * d + 8]
            fv = G[:, :, 16 * d + 8:16 * d + 16]
            S.op("act", lambda e, fv=fv: e.activation(out=v3(lf), in_=fv, func=AF.Sigmoid), reads=["G"], writes=["lf"])
            S.op("act", lambda e: e.activation(out=lf[:], in_=lf[:], func=AF.Ln), reads=["lf"], writes=["lf"])
            banks = [pst, pn[0], pn[1]]
            bkeys = ["pst", "pn0", "pn1"]
            lhs = [tri[:, d, :], tri[:, 2 + d, :], ones64[:]]
            for i in range(3):
                M = 128 if i == 2 else 64
                S.op("pe", lambda e, i=i, M=M, banks=banks, lhs=lhs: e.matmul(banks[i][:M, 0:NG], lhsT=lhs[i], rhs=lf[:], start=True, stop=True),
                     reads=["lf", "tri", "ones64"], writes=[bkeys[i]])
            S.op("dve", lambda e, iv=iv: e.tensor_tensor(out=v3(tmpg), in0=iv, in1=pst[0:64, 0:NG].rearrange("p (c h) -> p c h", h=8), op=ALU.subtract),
                 reads=["G", "pst"], writes=["tmpg"])
            S.op("act", lambda e, d=d: e.activation(out=u[d][:], in_=tmpg[:], func=AF.Exp), reads=["tmpg"], writes=["u%d" % d])
            S.op("dve", lambda e, d=d: e.tensor_scalar(out=u[d][:], in0=u[d][:], scalar1=0.0625, scalar2=None, op0=ALU.mult),
                 reads=["u%d" % d], writes=["u%d" % d])
            S.op("act", lambda e, d=d: e.activation(out=fl[d][:], in_=pst[0:64, 0:NG], func=AF.Exp, scale=-1.0),
                 reads=["pst"], writes=["fl%d" % d])
            S.op("dve", lambda e, iv=iv: e.tensor_tensor(out=v3(tmpg), in0=iv, in1=pn[0][0:64, 0:NG].rearrange("p (c h) -> p c h", h=8), op=ALU.add),
                 reads=["G", "pn0"], writes=["tmpg"])
            S.op("act", lambda e, d=d: e.activation(out=wsx[d][:], in_=tmpg[:], func=AF.Exp), reads=["tmpg"], writes=["ws%d" % d])
            S.op("dve", lambda e, d=d: e.tensor_scalar(out=wsx[d][:], in0=wsx[d][:], scalar1=0.0625, scalar2=None, op0=ALU.mult),
                 reads=["ws%d" % d], writes=["ws%d" % d])
            S.op("act", lambda e, d=d: e.activation(out=dec[d][:], in_=pn[1][:, 0:NG], func=AF.Exp),
                 reads=["pn1"], writes=["dec%d" % d])

        visit = [0]

        def chunk(c, d, full):
            vi = visit[0]
            visit[0] += 1
            sl = vi % 2
            kt_, kk = kTc[sl], "kTc%d" % sl
            qt_, qk = qTc[sl], "qTc%d" % sl
            vt_, vk = vch[sl], "vch%d" % sl
            kw_, kwk = kws[sl], "kws%d" % sl
            sm_, smk = sTm[sl], "sTm%d" % sl
            tsl = slice(c * 64, (c + 1) * 64)
            last_out = d == 1
            S.dma("sp", lambda e: e.dma_start(out=kt_[:], in_=T["kcT"][:, :, tsl]), writes=[kk])
            S.dma("sp", lambda e: e.dma_start(out=vt_[:], in_=T["vtok"][tsl, :]), writes=[vk])
            if full:
                S.dma("sp", lambda e: e.dma_start(out=qt_[:], in_=T["qcT"][:, :, tsl]), writes=[qk])
                if last_out:
                    S.dma("sp", lambda e: e.dma_start(out=soch[:], in_=T["sotok"][tsl, :]), writes=["soch"])
                    S.dma("sp", lambda e: e.dma_start(out=hfin[:], in_=T["hfwd"][tsl, :]), reads=[("hfwd", c)], writes=["hfin"])
            for i in range(16):
                S.op("pe", lambda e, i=i: e.transpose(ptrb[0:64, i * 128:(i + 1) * 128], kt_[:, i, :], ident[:]),
                     reads=[kk, "ident"], writes=["ptr"], signal=(i == 15))
            S.op("dve", lambda e: e.tensor_tensor(
                out=kw_[:], in0=ptrb[0:64, :].rearrange("p (h x) -> p h x", x=256),
                in1=wsx[d][:, c * 8:(c + 1) * 8].unsqueeze(2).to_broadcast([64, 8, 256]), op=ALU.mult),
                 reads=["ptr", "ws%d" % d], writes=[kwk])
            if full:
                for h in range(8):
                    for j in range(2):
                        S.op("pe", lambda e, h=h, j=j: e.matmul(pst[0:64, h * 64:(h + 1) * 64], lhsT=kt_[:, 2 * h + j, :], rhs=qt_[:, 2 * h + j, :],
                                                                start=(j == 0), stop=(j == 1)),
                             reads=[kk, qk], writes=["pst"], signal=(h == 7 and j == 1))
                S.op("dve", lambda e: e.tensor_tensor(
                    out=stf[:], in0=pst[0:64, :].rearrange("p (h t) -> p h t", t=64),
                    in1=u[d][:, c * 8:(c + 1) * 8].unsqueeze(2).to_broadcast([64, 8, 64]), op=ALU.mult),
                     reads=["pst", "u%d" % d], writes=["stf"])
                S.op("pool", lambda e: e.tensor_tensor(
                    out=sm_[:], in0=stf[:], in1=maskb[:, d, :].unsqueeze(1).to_broadcast([64, 8, 64]), op=ALU.mult),
                     reads=["stf", "maskb"], writes=[smk])
                for h in range(8):
                    for j in range(2):
                        S.op("pe", lambda e, h=h, j=j: e.matmul(pd[0:64, h:h + 1], lhsT=qt_[:, 2 * h + j, :], rhs=nb[:, h, j:j + 1],
                                                                start=(j == 0), stop=False),
                             reads=[qk, "nb"], writes=["pd"], signal=False)
                    S.op("pe", lambda e, h=h: e.matmul(pd[0:64, h:h + 1], lhsT=sm_[:, h, :], rhs=ones64b[:, 0:1], start=False, stop=True),
                         reads=[smk, "ones64b"], writes=["pd"], signal=(h == 7))
                S.op("dve", lambda e: e.tensor_scalar(out=rden[:], in0=pd[0:64, 0:8], scalar1=-1.0, scalar2=None, op0=ALU.mult),
                     reads=["pd"], writes=["rden"])
                S.op("dve", lambda e: e.tensor_tensor(out=rden[:], in0=rden[:], in1=pd[0:64, 0:8], op=ALU.max),
                     reads=["pd", "rden"], writes=["rden"])
                S.op("dve", lambda e: e.tensor_tensor(out=rden[:], in0=rden[:], in1=fl[d][:, c * 8:(c + 1) * 8], op=ALU.max),
                     reads=["rden", "fl%d" % d], writes=["rden"])
                S.op("dve", lambda e: e.reciprocal(out=rden[:], in_=rden[:]), reads=["rden"], writes=["rden"])
                for h in range(8):
                    pb, pbk = pn[h % 2], "pn%d" % (h % 2)
                    hs = slice(h * 512, (h + 1) * 512)
                    for j in range(2):
                        S.op("pe", lambda e, h=h, j=j, pb=pb: e.matmul(pb[0:64, :], lhsT=qt_[:, 2 * h + j, :], rhs=Cb[:, h, j, :],
                                                                       start=(j == 0), stop=False),
                             reads=[qk, "Cb"], writes=[pbk], signal=False)
                    S.op("pe", lambda e, h=h, pb=pb, hs=hs: e.matmul(pb[0:64, :], lhsT=sm_[:, h, :], rhs=vt_[:, hs], start=False, stop=True),
                         reads=[smk, vk], writes=[pbk])
                    if d == 0:
                        S.op("act", lambda e, h=h, pb=pb, hs=hs: e.activation(out=hsum[:, hs], in_=pb[0:64, :], func=AF.Copy, scale=rden[:, h:h + 1]),
                             reads=[pbk, "rden"], writes=["hsum"])
                    else:
                        S.op("dve", lambda e, h=h, pb=pb, hs=hs: e.scalar_tensor_tensor(
                            out=hsum[:, hs], in0=pb[0:64, :], scalar=rden[:, h:h + 1], in1=hfin[:, hs], op0=ALU.mult, op1=ALU.add),
                             reads=[pbk, "rden", "hfin"], writes=["hsum"])
            for h in range(8):
                hs = slice(h * 512, (h + 1) * 512)
                dcol = dec[d][:, c * 8 + h:c * 8 + h + 1]
                for j in range(2):
                    S.op("pe", lambda e, h=h, j=j, hs=hs: e.matmul(pu[j][:, :], lhsT=kw_[:, h, j * 128:(j + 1) * 128], rhs=vt_[:, hs], start=True, stop=True),
                         reads=[kwk, vk], writes=["pu%d" % j])
                    S.op("pe", lambda e, h=h, j=j: e.matmul(pd[:, 8 + 2 * h + j:9 + 2 * h + j], lhsT=kw_[:, h, j * 128:(j + 1) * 128], rhs=ones64b[:, 0:1],
                                                            start=True, stop=True),
                         reads=[kwk, "ones64b"], writes=["pd"])
                for j in range(2):
                    S.op("dve", lambda e, h=h, j=j, dcol=dcol: e.scalar_tensor_tensor(
                        out=C[:, h, j, :], in0=C[:, h, j, :], scalar=dcol, in1=pu[j][:, :], op0=ALU.mult, op1=ALU.add),
                         reads=["C", "pu%d" % j, "dec%d" % d], writes=["C"])
                S.op("act", lambda e, h=h: e.activation(out=Cb[:, h, :, :], in_=C[:, h, :, :], func=AF.Copy), reads=["C"], writes=["Cb"])
                S.op("dve", lambda e, h=h, dcol=dcol: e.scalar_tensor_tensor(
                    out=nst[:, h, :], in0=nst[:, h, :], scalar=dcol, in1=pd[:, 8 + 2 * h:10 + 2 * h], op0=ALU.mult, op1=ALU.add),
                     reads=["nst", "pd", "dec%d" % d], writes=["nst"])
                S.op("dve", lambda e, h=h: e.tensor_copy(out=nb[:, h, :], in_=nst[:, h, :]), reads=["nst"], writes=["nb"])
            if full and d == 0:
                S.dma("pool", lambda e: e.dma_start(out=T["hfwd"][tsl, :], in_=hsum[:]), reads=["hsum"], writes=[("hfwd", c)])
            if full and d == 1:
                for h in range(8):
                    hs = slice(h * 512, (h + 1) * 512)
                    S.op("act", lambda e, h=h, hs=hs: e.activation(out=sqj[:], in_=hsum[:, hs], func=AF.Square, accum_out=ss[:, h:h + 1]),
                         reads=["hsum"], writes=["sqj", "ss"])
                S.op("dve", lambda e: e.tensor_scalar(out=ss[:], in0=ss[:], scalar1=1.0 / 512, scalar2=RMS_EPS, op0=ALU.mult, op1=ALU.add),
                     reads=["ss"], writes=["ss"])
                S.op("act", lambda e: e.activation(out=ss[:], in_=ss[:], func=AF.Sqrt), reads=["ss"], writes=["ss"])
                S.op("dve", lambda e: e.reciprocal(out=ss[:], in_=ss[:]), reads=["ss"], writes=["ss"])
                for h in range(8):
                    hs = slice(h * 512, (h + 1) * 512)
                    S.op("dve", lambda e, h=h, hs=hs: e.scalar_tensor_tensor(
                        out=Abf[:, hs], in0=hsum[:, hs], scalar=ss[:, h:h + 1], in1=soch[:, hs], op0=ALU.mult, op1=ALU.mult),
                         reads=["hsum", "ss", "soch"], writes=["Abf"])
                for i in range(32):
                    S.op("pe", lambda e, i=i: e.transpose(ptrb[:, i * 64:(i + 1) * 64], Abf[:, i * 128:(i + 1) * 128], ident[0:64, 0:64]),
                         reads=["Abf", "ident"], writes=["ptr"], signal=(i == 31))
                q4 = c % 4
                S.op("dve", lambda e, q4=q4: e.tensor_tensor(
                    out=ATs[:, :, q4 * 64:(q4 + 1) * 64], in0=ptrb[:, :].rearrange("p (k t) -> p k t", t=64),
                    in1=hng[:].unsqueeze(2).to_broadcast([128, 32, 64]), op=ALU.mult),
                     reads=["ptr", "hng"], writes=["ATs"])
                if q4 == 0:
                    S.dma("pool", lambda e: e.dma_start(out=T["AT"][:, :, c * 64:c * 64 + 256], in_=ATs[:]),
                          reads=["ATs"], writes=[("AT", c)])

        def reset_state():
            S.op("dve", lambda e: e.memset(C[:], 0.0), writes=["C"])
            S.op("pool", lambda e: e.memset(Cb[:], 0.0), writes=["Cb"])
            S.op("dve", lambda e: e.memset(nst[:], 0.0), writes=["nst"])
            S.op("dve", lambda e: e.memset(nb[:], 0.0), writes=["nb"])

        reset_state()
        for c in range(n_own):
            chunk(c, 0, True)
        reset_state()
        for c in range(NCH - 1, n_own - 1, -1):
            chunk(c, 1, False)
        for c in range(n_own - 1, -1, -1):
            chunk(c, 1, True)
        S.end_phase()


def build_program(upto="all", moe_experts=32, mla_heads=32):
    nc = bass.Bass("TRN2", target_bir_lowering=False)
    stack = ExitStack()
    S = Sched(nc, stack)
    cx = Ctx(nc, S)
    T = {}

    def inp(name, shape, dt=F32):
        T[name] = nc.dram_tensor(name, list(shape), dt, kind="ExternalInput").ap()
        return T[name]

    def scr(name, shape, dt):
        T[name] = cx.scratch(name, shape, dt)
        return T[name]

    xT = inp("xT", [128, KC, SEQ])
    ln0g, ln0b = inp("ln0_g", [128, KC]), inp("ln0_b", [128, KC])
    inp("ident", [128, 128], BF16)
    inp("identf", [128, 128])
    inp("win_ch", [len(P1_CHUNKS), 128, KC, 128])
    inp("wg", [128, KC, 32])
    inp("bg", [128, 32])
    inp("convw", [128, 32, 3])
    inp("tri", [64, 4, 64])
    inp("hng", [128, 32])
    inp("wuq_h", [32, 128, 8, 256])
    inp("wukv_h", [32, 128, 4, 256])
    inp("qng", [128, 8])
    inp("kvng", [128, 4])
    inp("cosT", [64, SEQ])
    inp("sinT", [64, SEQ])
    for n in ("wpm_ch", "wpa_ch", "wout_ch"):
        inp(n, [KC, 128, KC, 128])
    for n in ("ln1_g", "ln1_b", "ln2_g", "ln2_b"):
        inp(n, [128, KC])
    inp("wr", [128, KC, 40])
    inp("br", [128, 40])
    inp("wge_ch", [32, 8, 128, KC, 128])
    inp("wue_ch", [32, 8, 128, KC, 128])
    inp("wde_ch", [32, KC, 128, 8, 128])

    xnT = scr("xnT", [128, KC, SEQ], BF16)
    x0T = scr("x0T", [128, KC, TOWN], F32)
    scr("qcT", [128, 16, TOWN], BF16)
    scr("kcT", [128, 16, SEQ], BF16)
    scr("vtok", [SEQ, 4096], BF16)
    scr("sotok", [TOWN, 4096], BF16)
    scr("gtok", [SEQ, 32], F32)
    scr("cqT", [128, 8, TOWN], BF16)
    scr("ckvT", [128, 4, SEQ], BF16)
    scr("krT", [64, 2, SEQ], BF16)
    scr("sgaT", [128, KC, TOWN], BF16)
    scr("sgbT", [128, KC, TOWN], BF16)
    scr("hfwd", [TOWN, 4096], F32)
    scr("AT", [128, KC, TOWN], BF16)
    scr("BT", [128, KC, TOWN], BF16)
    scr("mixaT", [128, KC, TOWN], F32)
    scr("mixT", [128, KC, TOWN], BF16)
    scr("z1T", [128, KC, TOWN], F32)
    scr("x1T", [128, KC, TOWN], F32)
    scr("x1bT", [128, KC, TOWN], BF16)
    scr("combT", [32, TOWN], F32)
    scr("moeT", [128, KC, TOWN], F32)
    T["outT"] = nc.dram_tensor("outT", [128, KC, TOWN], F32, kind="ExternalOutput").ap()

    Tb = 256
    with ExitStack() as st0:
        ybf = [st0.enter_context(nc.sbuf_tensor("p0ybf%d" % i, [128, KC, Tb], BF16)) for i in range(2)]

        def load_z(blk, zt, zk):
            S.dma("sp", lambda e: e.dma_start(out=zt[:], in_=xT[:, :, blk * Tb:(blk + 1) * Tb]), writes=[zk])

        def store_y(blk, yt, yk, st):
            sl = blk % 2
            S.op("pool", lambda e: e.tensor_copy(out=ybf[sl][:], in_=yt[:]), reads=[yk], writes=["ybf%d" % sl])
            S.dma("act", lambda e: e.dma_start(out=xnT[:, :, blk * Tb:(blk + 1) * Tb], in_=ybf[sl][:]),
                  reads=["ybf%d" % sl], writes=[("xnT", blk)])
            if (blk + 1) * Tb <= TOWN:
                S.dma("act", lambda e: e.dma_start(out=x0T[:, :, blk * Tb:(blk + 1) * Tb], in_=yt[:]),
                      reads=[yk], writes=[("x0T", blk)])

        ln_phase(cx, "p0", SEQ, Tb, load_z, ln0g, ln0b, store_y)

    stages = ["p0", "p1", "ml", "mla", "p4", "ln1", "rt", "moe", "all"]
    lvl = stages.index(upto)
    if lvl >= 1:
        p1_pass(cx, T, False)
        p1_pass(cx, T, True)
    if lvl >= 2:
        mlstm_phase(cx, T)
    if lvl >= 3:
        mla_phase(cx, T, mla_heads)
    if lvl >= 4:
        p4_phase(cx, T, "a")
        p4_phase(cx, T, "b")
        p4_phase(cx, T, "c")
    if lvl >= 5:
        with ExitStack() as st1:
            ybf = [st1.enter_context(nc.sbuf_tensor("l1ybf%d" % i, [128, KC, Tb], BF16)) for i in range(2)]

            def load_z1(blk, zt, zk):
                S.dma("sp", lambda e: e.dma_start(out=zt[:], in_=T["z1T"][:, :, blk * Tb:(blk + 1) * Tb]), writes=[zk])

            def store_y1(blk, yt, yk, st):
                sl = blk % 2
                S.op("pool", lambda e: e.tensor_copy(out=ybf[sl][:], in_=yt[:]), reads=[yk], writes=["ybf%d" % sl])
                S.dma("act", lambda e: e.dma_start(out=T["x1bT"][:, :, blk * Tb:(blk + 1) * Tb], in_=ybf[sl][:]),
                      reads=["ybf%d" % sl], writes=[("x1bT", blk)])
                S.dma("act", lambda e: e.dma_start(out=T["x1T"][:, :, blk * Tb:(blk + 1) * Tb], in_=yt[:]),
                      reads=[yk], writes=[("x1T", blk)])

            ln_phase(cx, "l1", TOWN, Tb, load_z1, T["ln1_g"], T["ln1_b"], store_y1)
    if lvl >= 6:
        router_phase(cx, T)
    if lvl >= 7:
        moe_phase(cx, T, moe_experts)
    if lvl >= 8:
        with ExitStack() as st2:
            mo = [st2.enter_context(nc.sbuf_tensor("l2mo%d" % i, [128, KC, Tb], F32)) for i in range(1)]

            def load_z2(blk, zt, zk):
                sl = 0
                S.dma("sp", lambda e: e.dma_start(out=zt[:], in_=T["x1T"][:, :, blk * Tb:(blk + 1) * Tb]), writes=[zk])
                S.dma("act", lambda e: e.dma_start(out=mo[sl][:], in_=T["moeT"][:, :, blk * Tb:(blk + 1) * Tb]), writes=["mo%d" % sl])
                S.op("dve", lambda e: e.scalar_tensor_tensor(out=zt[:], in0=zt[:], scalar=ALPHA, in1=mo[sl][:], op0=ALU.mult, op1=ALU.add),
                     reads=[zk, "mo%d" % sl], writes=[zk])

            def store_y2(blk, yt, yk, st):
                S.dma("act", lambda e: e.dma_start(out=T["outT"][:, :, blk * Tb:(blk + 1) * Tb], in_=yt[:]),
                      reads=[yk], writes=[("outT", blk)])

            ln_phase(cx, "l2", TOWN, Tb, load_z2, T["ln2_g"], T["ln2_b"], store_y2)

    stack.close()
    return nc


def fm(a2d):
    t, f = a2d.shape
    return np.ascontiguousarray(a2d.T.reshape(f // 128, 128, t).transpose(1, 0, 2))


def col128(v):
    return np.ascontiguousarray(v.reshape(-1, 128).T)


def wtile(w, cols):
    k = w.shape[0]
    out = np.zeros((128, k // 128, 128), np.float32)
    out[:, :, :len(cols)] = w[:, cols].reshape(k // 128, 128, len(cols)).transpose(1, 0, 2)
    return out


def wchunks(w):
    k, n = w.shape
    return np.ascontiguousarray(w.reshape(k // 128, 128, n // 128, 128).transpose(2, 1, 0, 3))


def shared_maps(inputs, stage="all"):
    import ml_dtypes
    f = lambda k: np.asarray(inputs[k], np.float32)
    w_in = f("w_in")[0]
    base = {"ln0_g": col128(f("ln0_g")), "ln0_b": col128(f("ln0_b")),
            "ident": np.eye(128, dtype=np.float32).astype(ml_dtypes.bfloat16),
            "identf": np.eye(128, dtype=np.float32)}
    wch = np.zeros((len(P1_CHUNKS), 128, KC, 128), np.float32)
    for n, (fam, fi, M) in enumerate(P1_CHUNKS):
        if fam == "kr":
            cols = W_OFF["kr"] + (np.arange(64) if fi == 0 else np.concatenate([np.arange(32, 64), np.arange(0, 32)]))
        else:
            cols = W_OFF[fam] + fi * 128 + np.arange(128)
        wch[n] = wtile(w_in, cols)
    base["win_ch"] = wch
    s_, t_ = np.meshgrid(np.arange(64), np.arange(64), indexing="ij")
    base["tri"] = np.ascontiguousarray(np.stack([s_ <= t_, s_ >= t_, s_ > t_, s_ < t_], 1).astype(np.float32))
    base["hng"] = col128(f("head_norm_g")[0])
    w_uq = f("w_uq")[0]
    wq = np.zeros((32, 128, 8, 256), np.float32)
    perm = np.concatenate([np.arange(32, 64), np.arange(0, 32)])
    for h in range(32):
        cols = np.concatenate([h * 192 + np.arange(192), h * 192 + 128 + perm])
        wq[h] = w_uq[:, cols].reshape(8, 128, 256).transpose(1, 0, 2)
    base["wuq_h"] = wq
    w_ukv = f("w_ukv")[0]
    base["wukv_h"] = np.ascontiguousarray(w_ukv.reshape(4, 128, 32, 256).transpose(2, 1, 0, 3))
    base["qng"] = col128(f("q_norm_g")[0])
    base["kvng"] = col128(f("kv_norm_g")[0])
    base["wpm_ch"] = wchunks(f("w_pm")[0])
    base["wpa_ch"] = wchunks(f("w_pa")[0])
    base["wout_ch"] = wchunks(f("w_out")[0])
    for n in ("ln1_g", "ln1_b", "ln2_g", "ln2_b"):
        base[n] = col128(f(n)[0])
    wr = np.concatenate([f("w_rg")[0], f("w_re")[0]], 1)
    base["wr"] = np.ascontiguousarray(wr.reshape(32, 128, 40).transpose(1, 0, 2))
    base["br"] = np.ascontiguousarray(np.broadcast_to(np.concatenate([f("b_rg")[0], f("b_re")[0]])[None, :], (128, 40)))
    base["wge_ch"] = np.ascontiguousarray(f("w_e_gate")[0].reshape(32, 32, 128, 8, 128).transpose(0, 3, 2, 1, 4))
    base["wue_ch"] = np.ascontiguousarray(f("w_e_up")[0].reshape(32, 32, 128, 8, 128).transpose(0, 3, 2, 1, 4))
    base["wde_ch"] = np.ascontiguousarray(f("w_e_down")[0].reshape(32, 8, 128, 32, 128).transpose(0, 3, 2, 1, 4))
    inv = 1.0 / (10000.0 ** (np.arange(0, 64, 2, dtype=np.float32) / 64.0))
    ang = np.arange(SEQ, dtype=np.float32)[:, None] * inv[None, :]
    cos, sin = np.cos(ang).astype(np.float32), np.sin(ang).astype(np.float32)
    cosT = np.concatenate([cos, cos], 1).T
    sinT = np.concatenate([-sin, sin], 1).T
    out = []
    for hf in range(2):
        m = dict(base)
        gperm = np.arange(32) if hf == 0 else np.concatenate([np.arange(16, 32), np.arange(0, 16)])
        m["wg"] = np.ascontiguousarray(wtile(w_in, W_OFF["g"] + gperm)[:, :, :32])
        m["bg"] = np.ascontiguousarray(np.broadcast_to(f("b_gates")[0][gperm][None, :], (128, 32)))
        cw = f("conv_qk")[0]
        if hf == 1:
            cw = cw[::-1]
        m["convw"] = np.ascontiguousarray(cw.T.reshape(32, 128, 3).transpose(1, 0, 2))
        m["cosT"] = np.ascontiguousarray(cosT if hf == 0 else cosT[:, ::-1])
        m["sinT"] = np.ascontiguousarray(sinT if hf == 0 else sinT[:, ::-1])
        out.append(m)
    return out


def make_in_maps(inputs):
    x = np.asarray(inputs["x"], dtype=np.float32)
    sh = shared_maps(inputs)
    maps = []
    for c in range(N_CORES):
        b, hf = c // 2, c % 2
        xb = x[b]
        if hf == 1:
            xb = xb[::-1]
        m = dict(sh[hf])
        m["xT"] = fm(xb)
        maps.append(m)
    return maps


def unpack_out(outT, hf):
    o = np.asarray(outT).transpose(2, 1, 0).reshape(TOWN, D)
    return o[::-1] if hf == 1 else o


def kernel(**inputs):
    nc = build_program()
    maps = make_in_maps(inputs)
    res = run_bass_kernel_spmd(nc, maps, core_ids=list(range(N_CORES)))
    x = inputs["x"]
    out = np.zeros(x.shape, np.float32)
    for c in range(N_CORES):
        b, hf = c // 2, c % 2
        out[b, hf * TOWN:(hf + 1) * TOWN] = unpack_out(res.results[c]["outT"], hf)
    return out
```

```python
import numpy as np
from contextlib import ExitStack

import concourse.bass as bass
import concourse.mybir as mybir
from concourse.bass_utils import run_bass_kernel_spmd

F32 = mybir.dt.float32
BF16 = mybir.dt.bfloat16
AF = mybir.ActivationFunctionType
ALU = mybir.AluOpType

D = 4096
KC = 32
SEQ = 4096
TOWN = 2048
N_CORES = 8
LN_EPS = 1e-5
RMS_EPS = 1e-6
ALPHA = 2.0 ** 0.25

DEBUG_OUT = set()


class _Q:
    def __init__(self, name, sem):
        self.name, self.sem, self.count = name, sem, 0
        self.ops, self.seen, self.pr, self.pw = [], {}, [], []


class Sched:
    BLK = {"pe": "tensor", "act": "scalar", "dve": "vector", "pool": "gpsimd", "sp": "sync"}

    def __init__(self, nc, stack, n_dma_sems=24):
        self.nc = nc
        self.q = {n: _Q(n, stack.enter_context(nc.semaphore("s_" + n))) for n in self.BLK}
        self.dsem = [[stack.enter_context(nc.semaphore("d%d" % i)), 0] for i in range(n_dma_sems)]
        self.dnext = 0
        self.lw, self.rd = {}, {}
        self.nops = 0

    def _wait(self, q, ev):
        sem, val = ev
        if q.seen.get(id(sem), 0) >= val:
            return
        q.seen[id(sem)] = val
        q.ops.append(("wait", sem, val))

    def _deps(self, q, reads, writes, is_pe):
        for k in reads:
            ev = self.lw.get(k)
            if ev is not None and not (is_pe and ev[0] is q.sem):
                self._wait(q, ev)
        for k in writes:
            ev = self.lw.get(k)
            if ev is not None and not (is_pe and ev[0] is q.sem):
                self._wait(q, ev)
            for ev in self.rd.get(k, ()):
                if ev[0] is q.sem:
                    continue
                self._wait(q, ev)

    def _record(self, ev, reads, writes):
        for k in reads:
            lst = self.rd.setdefault(k, [])
            lst[:] = [e for e in lst if e[0] is not ev[0]]
            lst.append(ev)
        for k in writes:
            self.lw[k] = ev
            self.rd[k] = []

    def op(self, eng, fn, reads=(), writes=(), signal=True):
        q = self.q[eng]
        self._deps(q, reads, writes, eng == "pe")
        self.nops += 1
        if signal:
            q.count += 1
            ev = (q.sem, q.count)
            q.ops.append(("op", fn, ev))
            self._record(ev, list(reads) + q.pr, list(writes) + q.pw)
            q.pr, q.pw = [], []
        else:
            q.ops.append(("op", fn, None))
            q.pr += list(reads)
            q.pw += list(writes)

    def dma(self, queue, fn, reads=(), writes=()):
        q = self.q[queue]
        slot = self.dsem[self.dnext]
        self.dnext = (self.dnext + 1) % len(self.dsem)
        if slot[1]:
            self._wait(q, (slot[0], slot[1]))
        self._deps(q, reads, writes, False)
        slot[1] += 16
        ev = (slot[0], slot[1])
        q.ops.append(("dma", fn, slot[0]))
        self._record(ev, reads, writes)
        self.nops += 1

    def end_phase(self):
        sp = self.q["sp"]
        for sem, val in self.dsem:
            if val:
                self._wait(sp, (sem, val))
        for n, q in self.q.items():
            assert not q.pr and not q.pw, "unsignalled tail on " + n
            if n != "sp" and q.count:
                self._wait(sp, (q.sem, q.count))
        with self.nc.Block() as block:
            for n, q in self.q.items():
                if not q.ops:
                    continue

                def body(e, ops=q.ops):
                    for o in ops:
                        if o[0] == "wait":
                            e.wait_ge(o[1], o[2])
                        elif o[0] == "op":
                            inst = o[1](e)
                            if o[2] is not None:
                                inst.then_inc(o[2][0], 1)
                        else:
                            o[1](e).then_inc(o[2], 16)

                getattr(block, self.BLK[n])(body)
                q.ops = []
        self.lw, self.rd = {}, {}


class Ctx:
    def __init__(self, nc, S):
        self.nc, self.S = nc, S
        self.dram = {}

    def scratch(self, name, shape, dtype):
        kind = "ExternalOutput" if name in DEBUG_OUT else "Internal"
        t = self.nc.dram_tensor(name, list(shape), dtype, kind=kind)
        self.dram[name] = t
        return t.ap()


def bc_mid(ap2d, n):
    return ap2d.unsqueeze(1).to_broadcast([ap2d.shape[0], n, ap2d.shape[1]])


def ln_phase(cx, tag, n_tok, Tb, load_z, g_dram, b_dram, store_y):
    nc, S = cx.nc, cx.S
    nblk = n_tok // Tb
    with ExitStack() as st:
        sb = lambda n, shp, dt: st.enter_context(nc.sbuf_tensor(tag + n, shp, dt))
        ps = lambda n, shp, dt: st.enter_context(nc.psum_tensor(tag + n, shp, dt))
        ones = sb("ones", [128, 128], F32)
        gcol = sb("g", [128, KC], F32)
        bcol = sb("b", [128, KC], F32)
        z = [sb("z%d" % i, [128, KC, Tb], F32) for i in range(2)]
        sq = sb("sq", [128, KC, Tb], F32)
        y = [sb("y%d" % i, [128, KC, Tb], F32) for i in range(2)]
        mean = sb("mean", [128, Tb], F32)
        m2 = sb("m2", [128, Tb], F32)
        var = sb("var", [128, Tb], F32)
        rstd = sb("rstd", [128, Tb], F32)
        cc = sb("cc", [128, Tb], F32)
        s1 = [ps("s1_%d" % i, [128, Tb], F32) for i in range(2)]
        s2 = [ps("s2_%d" % i, [128, Tb], F32) for i in range(2)]

        S.op("dve", lambda e: e.memset(ones[:], 1.0), writes=["ones"])
        S.dma("sp", lambda e: e.dma_start(out=gcol[:], in_=g_dram), writes=["gcol"])
        S.dma("sp", lambda e: e.dma_start(out=bcol[:], in_=b_dram), writes=["bcol"])

        def front(blk):
            sl = blk % 2
            zk, zt = "z%d" % sl, z[sl]
            s1t, s2t, s1k, s2k = s1[sl], s2[sl], "s1_%d" % sl, "s2_%d" % sl
            load_z(blk, zt, zk)
            S.op("pool", lambda e: e.tensor_tensor(out=sq[:], in0=zt[:], in1=zt[:], op=ALU.mult),
                 reads=[zk], writes=["sq"])
            for kc in range(KC):
                S.op("pe", lambda e, kc=kc: e.matmul(s1t[:], lhsT=ones[:], rhs=zt[:, kc, :],
                                                     start=(kc == 0), stop=(kc == KC - 1)),
                     reads=[zk, "ones"], writes=[s1k], signal=(kc == KC - 1))
            for kc in range(KC):
                S.op("pe", lambda e, kc=kc: e.matmul(s2t[:], lhsT=ones[:], rhs=sq[:, kc, :],
                                                     start=(kc == 0), stop=(kc == KC - 1)),
                     reads=["sq", "ones"], writes=[s2k], signal=(kc == KC - 1))

        def back(blk):
            sl = blk % 2
            zk, yk = "z%d" % sl, "y%d" % sl
            zt, yt = z[sl], y[sl]
            s1t, s2t, s1k, s2k = s1[sl], s2[sl], "s1_%d" % sl, "s2_%d" % sl
            S.op("dve", lambda e: e.tensor_scalar(out=mean[:], in0=s1t[:], scalar1=1.0 / D, scalar2=None,
                                                  op0=ALU.mult), reads=[s1k], writes=["mean"])
            S.op("dve", lambda e: e.tensor_tensor(out=m2[:], in0=mean[:], in1=mean[:], op=ALU.mult),
                 reads=["mean"], writes=["m2"])
            S.op("dve", lambda e: e.scalar_tensor_tensor(out=var[:], in0=s2t[:], scalar=1.0 / D, in1=m2[:],
                                                         op0=ALU.mult, op1=ALU.subtract),
                 reads=[s2k, "m2"], writes=["var"])
            S.op("dve", lambda e: e.tensor_scalar(out=var[:], in0=var[:], scalar1=LN_EPS, scalar2=None,
                                                  op0=ALU.add), reads=["var"], writes=["var"])
            S.op("act", lambda e: e.activation(out=var[:], in_=var[:], func=AF.Sqrt),
                 reads=["var"], writes=["var"])
            S.op("dve", lambda e: e.reciprocal(out=rstd[:], in_=var[:]), reads=["var"], writes=["rstd"])
            S.op("dve", lambda e: e.scalar_tensor_tensor(out=cc[:], in0=mean[:], scalar=-1.0, in1=rstd[:],
                                                         op0=ALU.mult, op1=ALU.mult),
                 reads=["mean", "rstd"], writes=["cc"])
            S.op("dve", lambda e: e.tensor_tensor(out=zt[:], in0=zt[:], in1=bc_mid(rstd[:], KC), op=ALU.mult),
                 reads=[zk, "rstd"], writes=[zk])
            S.op("dve", lambda e: e.tensor_tensor(out=zt[:], in0=zt[:], in1=bc_mid(cc[:], KC), op=ALU.add),
                 reads=[zk, "cc"], writes=[zk])
            for kc in range(KC):
                S.op("act", lambda e, kc=kc: e.activation(
                    out=yt[:, kc, :], in_=zt[:, kc, :], func=AF.Identity,
                    bias=bcol[:, kc:kc + 1], scale=gcol[:, kc:kc + 1]),
                     reads=[zk, "gcol", "bcol"], writes=[yk], signal=(kc == KC - 1))
            store_y(blk, yt, yk, st)

        front(0)
        for blk in range(nblk):
            if blk + 1 < nblk:
                front(blk + 1)
            back(blk)
        S.end_phase()


W_OFF = dict(q=0, k=2048, v=4096, o=8192, g=12288, cq=12320, ckv=13344, kr=13856, ga=13920, gb=18016)
P1_CHUNKS = ([("q", i, 128) for i in range(16)] + [("k", i, 128) for i in range(16)] +
             [("cq", i, 128) for i in range(8)] + [("ckv", i, 128) for i in range(4)] +
             [("kr", i, 64) for i in range(2)] + [("v", i, 128) for i in range(32)] +
             [("o", i, 128) for i in range(32)] + [("ga", i, 128) for i in range(32)] +
             [("gb", i, 128) for i in range(32)])
P1_IDX = {(f, i): n for n, (f, i, m) in enumerate(P1_CHUNKS)}
NRES = TOWN + 2


def p1_pass(cx, T, pass_b):
    nc, S = cx.nc, cx.S
    tag = "p1b" if pass_b else "p1a"
    tok0 = (SEQ - NRES) if pass_b else 0
    doff = 2 if pass_b else 0
    otok0 = TOWN if pass_b else 0
    fams = ("k", "ckv", "kr", "v") if pass_b else ("q", "k", "cq", "ckv", "kr", "v", "o", "ga", "gb")
    chunks = [c for c in P1_CHUNKS if c[0] in fams]
    with ExitStack() as st:
        sb = lambda n, shp, dt: st.enter_context(nc.sbuf_tensor(tag + n, shp, dt))
        ps = lambda n, shp, dt: st.enter_context(nc.psum_tensor(tag + n, shp, dt))
        act = sb("act", [128, KC, NRES], BF16)
        NW = 3
        wb = [sb("w%d" % i, [128, KC, 128], BF16) for i in range(NW)]
        raw = sb("raw", [128, NRES + 2], F32)
        cv = sb("cv", [128, TOWN], F32)
        ob = [sb("ob%d" % i, [128, TOWN], BF16) for i in range(2)]
        vtk = [sb("vtk%d" % i, [128, 16, 128], BF16) for i in range(2)]
        ident = sb("ident", [128, 128], BF16)
        convw = sb("convw", [128, 32, 3], F32)
        wg = sb("wg", [128, KC, 32], BF16)
        bg = sb("bg", [128, 32], F32)
        gout = sb("gout", [128, 16, 32], F32)
        pm = [ps("pm%d" % i, [128, 512], F32) for i in range(4)]
        ph = ps("ph", [128, 512], F32)
        ptr = ps("ptr", [128, 16, 128], BF16)

        S.dma("sp", lambda e: e.dma_start(out=ident[:], in_=T["ident"]), writes=["ident"])
        S.dma("sp", lambda e: e.dma_start(out=convw[:], in_=T["convw"]), writes=["convw"])
        S.dma("sp", lambda e: e.dma_start(out=bg[:], in_=T["bg"]), writes=["bg"])
        S.dma("pool", lambda e: e.dma_start(out=wg[:], in_=T["wg"], max_dma_last_dim=4096), writes=["wg"])
        for kq in range(4):
            S.dma("sp" if kq % 2 == 0 else "act",
                  lambda e, kq=kq: e.dma_start(out=act[:, kq * 8:(kq + 1) * 8, :],
                                               in_=T["xnT"][:, kq * 8:(kq + 1) * 8, tok0:tok0 + NRES]),
                  writes=[("act", kq)])
        actk = [("act", kq) for kq in range(4)]
        S.op("dve", lambda e: e.memset(raw[:], 0.0), writes=["raw"])

        deferred = []

        def run_deferred():
            for f in deferred:
                f()
            deferred.clear()

        evac_flip = [0]

        def evac(out_ap, in_ap, reads, writes, func=None):
            if func is not None:
                S.op("act", lambda e: e.activation(out=out_ap, in_=in_ap, func=func), reads=reads, writes=writes)
                return
            evac_flip[0] ^= 1
            if evac_flip[0]:
                S.op("act", lambda e: e.activation(out=out_ap, in_=in_ap, func=AF.Copy), reads=reads, writes=writes)
            else:
                S.op("dve", lambda e: e.tensor_copy(out=out_ap, in_=in_ap), reads=reads, writes=writes)

        for ci, (fam, fi, M) in enumerate(chunks):
            slot = ci % NW
            wk = "w%d" % slot
            wt = wb[slot]
            gidx = P1_IDX[(fam, fi)]
            S.dma("pool", lambda e, wt=wt, gidx=gidx: e.dma_start(out=wt[:], in_=T["win_ch"][gidx],
                                                                  max_dma_last_dim=4096), writes=[wk])
            halo = fam in ("q", "k")
            boff = 0 if halo else doff
            for tb in range(4):
                for kc in range(KC):
                    S.op("pe", lambda e, wt=wt, kc=kc, tb=tb, boff=boff, M=M: e.matmul(
                        pm[tb][:M, :], lhsT=wt[:, kc, :M], rhs=act[:, kc, boff + tb * 512: boff + (tb + 1) * 512],
                        start=(kc == 0), stop=(kc == KC - 1)),
                         reads=[wk] + actk, writes=["pm%d" % tb], signal=(kc == KC - 1))
            if halo:
                for kc in range(KC):
                    S.op("pe", lambda e, wt=wt, kc=kc: e.matmul(
                        ph[:, 0:2], lhsT=wt[:, kc, :], rhs=act[:, kc, TOWN:TOWN + 2],
                        start=(kc == 0), stop=(kc == KC - 1)),
                         reads=[wk] + actk, writes=["ph"], signal=(kc == KC - 1))
            run_deferred()

            if halo:
                d0 = 0 if pass_b else 1
                c0 = 1 if pass_b else 0
                for tb in range(4):
                    evac(raw[:, d0 + tb * 512: d0 + (tb + 1) * 512], pm[tb][:, :], ["pm%d" % tb], ["raw"])
                evac(raw[:, d0 + TOWN: d0 + TOWN + 2], ph[:, 0:2], ["ph"], ["raw"])
                cw = (0 if fam == "q" else 16) + fi
                S.op("dve", lambda e, c0=c0, cw=cw: e.tensor_scalar(
                    out=cv[:], in0=raw[:, c0:c0 + TOWN], scalar1=convw[:, cw, 0:1], scalar2=None, op0=ALU.mult),
                     reads=["raw", "convw"], writes=["cv"])
                S.op("dve", lambda e, c0=c0, cw=cw: e.scalar_tensor_tensor(
                    out=cv[:], in0=raw[:, c0 + 1:c0 + 1 + TOWN], scalar=convw[:, cw, 1:2], in1=cv[:],
                    op0=ALU.mult, op1=ALU.add), reads=["raw", "convw", "cv"], writes=["cv"])
                S.op("dve", lambda e, c0=c0, cw=cw: e.scalar_tensor_tensor(
                    out=cv[:], in0=raw[:, c0 + 2:c0 + 2 + TOWN], scalar=convw[:, cw, 2:3], in1=cv[:],
                    op0=ALU.mult, op1=ALU.add), reads=["raw", "convw", "cv"], writes=["cv"])
                osl = ci % 2
                S.op("act", lambda e, osl=osl: e.activation(out=ob[osl][:], in_=cv[:], func=AF.Silu),
                     reads=["cv"], writes=["ob%d" % osl])
                dst = T["qcT"] if fam == "q" else T["kcT"]
                S.dma("sp", lambda e, osl=osl, dst=dst, fi=fi: e.dma_start(
                    out=dst[:, fi, otok0:otok0 + TOWN], in_=ob[osl][:]),
                      reads=["ob%d" % osl], writes=[(fam, fi, pass_b)])
            elif fam in ("cq", "ckv", "kr", "ga", "gb"):
                osl = ci % 2
                func = AF.Sigmoid if fam in ("ga", "gb") else None
                for tb in range(4):
                    evac(ob[osl][:M, tb * 512:(tb + 1) * 512], pm[tb][:M, :], ["pm%d" % tb], ["ob%d" % osl], func)
                dst = {"cq": "cqT", "ckv": "ckvT", "kr": "krT", "ga": "sgaT", "gb": "sgbT"}[fam]
                S.dma("sp", lambda e, osl=osl, dst=dst, fi=fi, M=M: e.dma_start(
                    out=T[dst][:M, fi, otok0:otok0 + TOWN], in_=ob[osl][:M, :]),
                      reads=["ob%d" % osl], writes=[(fam, fi, pass_b)])
            else:
                osl = ci % 2
                func = AF.Sigmoid if fam == "o" else None
                for tb in range(4):
                    evac(ob[osl][:, tb * 512:(tb + 1) * 512], pm[tb][:, :], ["pm%d" % tb], ["ob%d" % osl], func)

                def tr(osl=osl, fam=fam, fi=fi):
                    for j in range(16):
                        S.op("pe", lambda e, j=j: e.transpose(ptr[:, j, :], ob[osl][:, j * 128:(j + 1) * 128], ident[:]),
                             reads=["ob%d" % osl, "ident"], writes=["ptr"], signal=(j == 15))
                    S.op("dve", lambda e: e.tensor_copy(out=vtk[osl][:], in_=ptr[:]), reads=["ptr"],
                         writes=["vtk%d" % osl])
                    dst = T["vtok"] if fam == "v" else T["sotok"]
                    S.dma("act", lambda e: e.dma_start(
                        out=dst[otok0:otok0 + TOWN, fi * 128:(fi + 1) * 128].rearrange("(j p) c -> p j c", p=128),
                        in_=vtk[osl][:]), reads=["vtk%d" % osl], writes=[(fam, fi, pass_b)])
                deferred.append(tr)
        run_deferred()

        for tt in range(16):
            for kc in range(KC):
                S.op("pe", lambda e, tt=tt, kc=kc: e.matmul(
                    ph[:, 32:64], lhsT=act[:, kc, doff + tt * 128: doff + (tt + 1) * 128], rhs=wg[:, kc, :],
                    start=(kc == 0), stop=(kc == KC - 1)),
                     reads=["wg"] + actk, writes=["phg"], signal=(kc == KC - 1))
            S.op("dve", lambda e, tt=tt: e.tensor_tensor(out=gout[:, tt, :], in0=ph[:, 32:64], in1=bg[:], op=ALU.add),
                 reads=["phg", "bg"], writes=["gout"])
        S.dma("sp", lambda e: e.dma_start(
            out=T["gtok"][otok0:otok0 + TOWN, :].rearrange("(j p) c -> p j c", p=128), in_=gout[:]),
              reads=["gout"], writes=[("gtok", pass_b)])
        S.end_phase()


def mm_chunk(S, pm, pmk, wt, wk, act, actk, kcn, ntb, M=128, toff=0):
    for tb in range(ntb):
        for kc in range(kcn):
            S.op("pe", lambda e, tb=tb, kc=kc: e.matmul(
                pm[tb][:M, :], lhsT=wt[:, kc, :M], rhs=act[:, kc, toff + tb * 512: toff + (tb + 1) * 512],
                start=(kc == 0), stop=(kc == kcn - 1)),
                 reads=[wk] + actk, writes=[pmk[tb]], signal=(kc == kcn - 1))


def load_resident(S, act, src, kcn, key):
    n = 4 if kcn >= 4 else 1
    step = kcn // n
    keys = []
    for i in range(n):
        S.dma("sp" if i % 2 == 0 else "act",
              lambda e, i=i: e.dma_start(out=act[:, i * step:(i + 1) * step, :], in_=src[:, i * step:(i + 1) * step, :]),
              writes=[(key, i)])
        keys.append((key, i))
    return keys


def p4_phase(cx, T, which):
    nc, S = cx.nc, cx.S
    tag = "p4" + which
    src = {"a": "AT", "b": "BT", "c": "mixT"}[which]
    wname = {"a": "wpm_ch", "b": "wpa_ch", "c": "wout_ch"}[which]
    with ExitStack() as st:
        sb = lambda n, shp, dt: st.enter_context(nc.sbuf_tensor(tag + n, shp, dt))
        ps = lambda n, shp, dt: st.enter_context(nc.psum_tensor(tag + n, shp, dt))
        act = sb("act", [128, KC, TOWN], BF16)
        NW = 3
        wb = [sb("w%d" % i, [128, KC, 128], BF16) for i in range(NW)]
        e1 = [sb("e1_%d" % i, [128, 512], F32) for i in range(3)]
        e2 = [sb("e2_%d" % i, [128, 512], F32 if which == "c" else BF16) for i in range(3)]
        o1 = [sb("o1_%d" % i, [128, 512], F32) for i in range(3)]
        o2 = [sb("o2_%d" % i, [128, 512], BF16) for i in range(3)]
        pm = [ps("pm%d" % i, [128, 512], F32) for i in range(8)]
        actk = load_resident(S, act, T[src], KC, "act")
        n = 0
        for ci in range(KC):
            slot = ci % NW
            wt, wk = wb[slot], "w%d" % slot
            S.dma("pool", lambda e, wt=wt, ci=ci: e.dma_start(out=wt[:], in_=T[wname][ci], max_dma_last_dim=4096),
                  writes=[wk])
            pset = [pm[(ci % 2) * 4 + i] for i in range(4)]
            pkey = ["pm%d" % ((ci % 2) * 4 + i) for i in range(4)]
            mm_chunk(S, pset, pkey, wt, wk, act, actk, KC, 4)
            for tb in range(4):
                b = n % 3
                n += 1
                tsl = slice(tb * 512, (tb + 1) * 512)
                if which == "a":
                    S.dma("sp", lambda e, b=b, ci=ci, tsl=tsl: e.dma_start(out=e2[b][:], in_=T["sgaT"][:, ci, tsl]),
                          writes=["e2_%d" % b])
                    S.op("dve", lambda e, b=b, tb=tb, pset=pset: e.tensor_tensor(out=o1[b][:], in0=pset[tb][:], in1=e2[b][:], op=ALU.mult),
                         reads=[pkey[tb], "e2_%d" % b], writes=["o1_%d" % b])
                    S.dma("act", lambda e, b=b, ci=ci, tsl=tsl: e.dma_start(out=T["mixaT"][:, ci, tsl], in_=o1[b][:]),
                          reads=["o1_%d" % b], writes=[("mixaT", ci, tb)])
                elif which == "b":
                    S.dma("sp", lambda e, b=b, ci=ci, tsl=tsl: e.dma_start(out=e2[b][:], in_=T["sgbT"][:, ci, tsl]),
                          writes=["e2_%d" % b])
                    S.dma("sp", lambda e, b=b, ci=ci, tsl=tsl: e.dma_start(out=e1[b][:], in_=T["mixaT"][:, ci, tsl]),
                          writes=["e1_%d" % b])
                    S.op("dve", lambda e, b=b, tb=tb, pset=pset: e.tensor_tensor(out=o1[b][:], in0=pset[tb][:], in1=e2[b][:], op=ALU.mult),
                         reads=[pkey[tb], "e2_%d" % b], writes=["o1_%d" % b])
                    S.op("dve", lambda e, b=b: e.tensor_tensor(out=o2[b][:], in0=o1[b][:], in1=e1[b][:], op=ALU.add),
                         reads=["o1_%d" % b, "e1_%d" % b], writes=["o2_%d" % b])
                    S.dma("act", lambda e, b=b, ci=ci, tsl=tsl: e.dma_start(out=T["mixT"][:, ci, tsl], in_=o2[b][:]),
                          reads=["o2_%d" % b], writes=[("mixT", ci, tb)])
                else:
                    S.dma("sp", lambda e, b=b, ci=ci, tsl=tsl: e.dma_start(out=e2[b][:], in_=T["x0T"][:, ci, tsl]),
                          writes=["e2_%d" % b])
                    S.op("dve", lambda e, b=b, tb=tb, pset=pset: e.scalar_tensor_tensor(
                        out=o1[b][:], in0=e2[b][:], scalar=ALPHA, in1=pset[tb][:], op0=ALU.mult, op1=ALU.add),
                         reads=[pkey[tb], "e2_%d" % b], writes=["o1_%d" % b])
                    S.dma("act", lambda e, b=b, ci=ci, tsl=tsl: e.dma_start(out=T["z1T"][:, ci, tsl], in_=o1[b][:]),
                          reads=["o1_%d" % b], writes=[("z1T", ci, tb)])
        S.end_phase()


def router_phase(cx, T):
    nc, S = cx.nc, cx.S
    tag = "rt"
    BIG = 1.0e4
    with ExitStack() as st:
        sb = lambda n, shp, dt: st.enter_context(nc.sbuf_tensor(tag + n, shp, dt))
        ps = lambda n, shp, dt: st.enter_context(nc.psum_tensor(tag + n, shp, dt))
        wr = sb("wr", [128, KC, 40], F32)
        br = sb("br", [128, 40], F32)
        identf = sb("identf", [128, 128], F32)
        xf = [sb("xf%d" % i, [128, KC, 128], F32) for i in range(2)]
        L = sb("L", [128, 40], F32)
        em = sb("em", [128, 32], F32)
        em2 = sb("em2", [128, 32], F32)
        m1 = sb("m1", [128, 32], F32)
        m2 = sb("m2", [128, 32], F32)
        comb = sb("comb", [128, 32], F32)
        gmask = sb("gmask", [128, 8], F32)
        ex = sb("ex", [128, 8], F32)
        sc = sb("sc", [128, 16], F32)
        combT = sb("combT", [32, TOWN], F32)
        pl = ps("pl", [128, 512], F32)
        pt = ps("pt", [128, 512], F32)
        S.dma("sp", lambda e: e.dma_start(out=wr[:], in_=T["wr"]), writes=["wr"])
        S.dma("sp", lambda e: e.dma_start(out=br[:], in_=T["br"]), writes=["br"])
        S.dma("sp", lambda e: e.dma_start(out=identf[:], in_=T["identf"]), writes=["identf"])
        c = lambda i: sc[:, i:i + 1]
        for tt in range(TOWN // 128):
            sl = tt % 2
            xk = "xf%d" % sl
            S.dma("sp" if sl == 0 else "act",
                  lambda e, sl=sl, tt=tt: e.dma_start(out=xf[sl][:], in_=T["x1T"][:, :, tt * 128:(tt + 1) * 128]),
                  writes=[xk])
            for kc in range(KC):
                S.op("pe", lambda e, sl=sl, kc=kc: e.matmul(pl[:, 0:40], lhsT=xf[sl][:, kc, :], rhs=wr[:, kc, :],
                                                            start=(kc == 0), stop=(kc == KC - 1)),
                     reads=[xk, "wr"], writes=["pl"], signal=(kc == KC - 1))
            dv = lambda fn, r, w: S.op("dve", fn, reads=r, writes=w)
            dv(lambda e: e.tensor_tensor(out=L[:], in0=pl[:, 0:40], in1=br[:], op=ALU.add), ["pl", "br"], ["L"])
            dv(lambda e: e.tensor_reduce(out=c(0), in_=L[:, 0:8], axis=mybir.AxisListType.X, op=ALU.max), ["L"], ["sc"])
            dv(lambda e: e.tensor_scalar(out=gmask[:], in0=L[:, 0:8], scalar1=c(0), scalar2=None, op0=ALU.is_equal),
               ["L", "sc"], ["gmask"])
            dv(lambda e: e.tensor_scalar(out=c(1), in0=c(0), scalar1=-1.0, scalar2=None, op0=ALU.mult), ["sc"], ["sc"])
            S.op("act", lambda e: e.activation(out=ex[:], in_=L[:, 0:8], func=AF.Exp, bias=c(1), scale=1.0,
                                               accum_out=c(2)), reads=["L", "sc"], writes=["ex", "sc"])
            dv(lambda e: e.reciprocal(out=c(3), in_=c(2)), ["sc"], ["sc"])
            dv(lambda e: e.tensor_scalar(out=gmask[:], in0=gmask[:], scalar1=BIG, scalar2=-BIG, op0=ALU.mult, op1=ALU.add),
               ["gmask"], ["gmask"])
            dv(lambda e: e.tensor_tensor(out=em[:].rearrange("p (g x) -> p g x", x=4),
                                         in0=L[:, 8:40].rearrange("p (g x) -> p g x", x=4),
                                         in1=gmask[:].unsqueeze(2).to_broadcast([128, 8, 4]), op=ALU.add),
               ["L", "gmask"], ["em"])
            dv(lambda e: e.tensor_reduce(out=c(4), in_=em[:], axis=mybir.AxisListType.X, op=ALU.max), ["em"], ["sc"])
            dv(lambda e: e.tensor_scalar(out=m1[:], in0=em[:], scalar1=c(4), scalar2=None, op0=ALU.is_equal),
               ["em", "sc"], ["m1"])
            dv(lambda e: e.scalar_tensor_tensor(out=em2[:], in0=m1[:], scalar=-BIG, in1=em[:], op0=ALU.mult, op1=ALU.add),
               ["m1", "em"], ["em2"])
            dv(lambda e: e.tensor_reduce(out=c(5), in_=em2[:], axis=mybir.AxisListType.X, op=ALU.max), ["em2"], ["sc"])
            dv(lambda e: e.tensor_scalar(out=m2[:], in0=em2[:], scalar1=c(5), scalar2=None, op0=ALU.is_equal),
               ["em2", "sc"], ["m2"])
            dv(lambda e: e.tensor_tensor(out=c(6), in0=c(5), in1=c(4), op=ALU.subtract), ["sc"], ["sc"])
            S.op("act", lambda e: e.activation(out=c(7), in_=c(6), func=AF.Exp), reads=["sc"], writes=["sc"])
            dv(lambda e: e.tensor_scalar(out=c(8), in0=c(7), scalar1=1.0, scalar2=None, op0=ALU.add), ["sc"], ["sc"])
            dv(lambda e: e.reciprocal(out=c(8), in_=c(8)), ["sc"], ["sc"])
            dv(lambda e: e.tensor_tensor(out=c(9), in0=c(7), in1=c(8), op=ALU.mult), ["sc"], ["sc"])
            dv(lambda e: e.tensor_tensor(out=c(10), in0=c(8), in1=c(3), op=ALU.mult), ["sc"], ["sc"])
            dv(lambda e: e.tensor_tensor(out=c(11), in0=c(9), in1=c(3), op=ALU.mult), ["sc"], ["sc"])
            dv(lambda e: e.tensor_scalar(out=comb[:], in0=m1[:], scalar1=c(10), scalar2=None, op0=ALU.mult),
               ["m1", "sc"], ["comb"])
            dv(lambda e: e.scalar_tensor_tensor(out=comb[:], in0=m2[:], scalar=c(11), in1=comb[:], op0=ALU.mult, op1=ALU.add),
               ["m2", "sc", "comb"], ["comb"])
            S.op("pe", lambda e: e.transpose(pt[0:32, 0:128], comb[:], identf[:]), reads=["comb", "identf"], writes=["pt"])
            S.op("act", lambda e, tt=tt: e.activation(out=combT[:, tt * 128:(tt + 1) * 128], in_=pt[0:32, 0:128], func=AF.Copy),
                 reads=["pt"], writes=["combT"])
        S.dma("sp", lambda e: e.dma_start(out=T["combT"], in_=combT[:]), reads=["combT"], writes=["combT_d"])
        S.end_phase()


def moe_phase(cx, T, n_experts=32):
    nc, S = cx.nc, cx.S
    tag = "moe"
    TS = 1024
    with ExitStack() as st:
        sb = lambda n, shp, dt: st.enter_context(nc.sbuf_tensor(tag + n, shp, dt))
        ps = lambda n, shp, dt: st.enter_context(nc.psum_tensor(tag + n, shp, dt))
        act = sb("act", [128, KC, TS], BF16)
        hT = [sb("hT%d" % i, [128, 8, TS], BF16) for i in range(2)]
        NW = 4
        wb = [sb("w%d" % i, [128, KC, 128], BF16) for i in range(NW)]
        wd = [sb("wd%d" % i, [128, 8, 128], BF16) for i in range(NW)]
        sg = [sb("sg%d" % i, [128, TS], BF16) for i in range(2)]
        sgc = [sb("sgc%d" % i, [128, TS], F32) for i in range(2)]
        cb = [sb("cb%d" % i, [128, TS], F32) for i in range(2)]
        cmask = sb("cmask", [32, TS], F32)
        combT = sb("combT", [32, TOWN], F32)
        ident32 = sb("ident32", [32, 32], F32)
        ones32 = sb("ones32", [32, 128], F32)
        ai = [sb("ai%d" % i, [128, TS], F32) for i in range(2)]
        ao = [sb("ao%d" % i, [128, TS], F32) for i in range(2)]
        pm = [ps("pm%d" % i, [128, 512], F32) for i in range(8)]
        S.dma("sp", lambda e: e.dma_start(out=combT[:], in_=T["combT"]), writes=["combT"])
        S.dma("sp", lambda e: e.dma_start(out=ident32[:], in_=T["identf"][0:32, 0:32]), writes=["ident32"])
        S.op("dve", lambda e: e.memset(ones32[:], 1.0), writes=["ones32"])
        wcnt = [0, 0]
        acnt = [0]

        def gate_up(s, e_, hsl):
            tsl = slice(s * TS, (s + 1) * TS)
            csl = e_ % 2
            S.op("dve", lambda e: e.tensor_scalar(out=cmask[:], in0=combT[:, tsl], scalar1=ident32[:, e_:e_ + 1],
                                                  scalar2=None, op0=ALU.mult),
                 reads=["combT", "ident32"], writes=["cmask"])
            for tb in range(2):
                S.op("pe", lambda e, tb=tb: e.matmul(pm[6 + tb][:, :], lhsT=ones32[:], rhs=cmask[:, tb * 512:(tb + 1) * 512],
                                                     start=True, stop=True),
                     reads=["cmask", "ones32"], writes=["pm%d" % (6 + tb)])
                S.op("act", lambda e, tb=tb: e.activation(out=cb[csl][:, tb * 512:(tb + 1) * 512], in_=pm[6 + tb][:, :], func=AF.Copy),
                     reads=["pm%d" % (6 + tb)], writes=["cb%d" % csl])
            for j in range(8):
                for which in range(2):
                    slot = wcnt[0] % NW
                    wcnt[0] += 1
                    wt, wk = wb[slot], "w%d" % slot
                    src = T["wge_ch"] if which == 0 else T["wue_ch"]
                    S.dma("pool", lambda e, wt=wt, src=src, j=j: e.dma_start(out=wt[:], in_=src[e_, j], max_dma_last_dim=4096),
                          writes=[wk])
                    pset = [pm[which * 2 + i] for i in range(2)]
                    pkey = ["pm%d" % (which * 2 + i) for i in range(2)]
                    mm_chunk(S, pset, pkey, wt, wk, act, actk, KC, 2)
                    gsl = j % 2
                    if which == 0:
                        for tb in range(2):
                            S.op("act", lambda e, tb=tb, pset=pset, gsl=gsl: e.activation(
                                out=sg[gsl][:, tb * 512:(tb + 1) * 512], in_=pset[tb][:, :], func=AF.Silu),
                                 reads=[pkey[tb]], writes=["sg%d" % gsl], signal=(tb == 1))
                        S.op("dve", lambda e, gsl=gsl: e.tensor_tensor(out=sgc[gsl][:], in0=sg[gsl][:], in1=cb[csl][:], op=ALU.mult),
                             reads=["sg%d" % gsl, "cb%d" % csl], writes=["sgc%d" % gsl])
                    else:
                        for tb in range(2):
                            S.op("dve", lambda e, tb=tb, pset=pset, gsl=gsl, j=j: e.tensor_tensor(
                                out=hT[hsl][:, j, tb * 512:(tb + 1) * 512], in0=pset[tb][:, :],
                                in1=sgc[gsl][:, tb * 512:(tb + 1) * 512], op=ALU.mult),
                                 reads=[pkey[tb], "sgc%d" % gsl], writes=["hT%d" % hsl])

        def down(s, e_, hsl, first):
            for c in range(KC):
                slot = wcnt[1] % NW
                wcnt[1] += 1
                wt, wk = wd[slot], "wd%d" % slot
                S.dma("pool", lambda e, wt=wt, c=c: e.dma_start(out=wt[:], in_=T["wde_ch"][e_, c], max_dma_last_dim=4096),
                      writes=[wk])
                pset = [pm[4 + i] for i in range(2)]
                pkey = ["pm%d" % (4 + i) for i in range(2)]
                mm_chunk(S, pset, pkey, wt, wk, hT[hsl], ["hT%d" % hsl], 8, 2)
                b = acnt[0] % 2
                acnt[0] += 1
                dst = T["moeT"][:, c, s * TS:(s + 1) * TS]
                if first:
                    for tb in range(2):
                        S.op("dve", lambda e, tb=tb, b=b: e.tensor_copy(out=ao[b][:, tb * 512:(tb + 1) * 512], in_=pset[tb][:, :]),
                             reads=[pkey[tb]], writes=["ao%d" % b], signal=(tb == 1))
                else:
                    S.dma("act", lambda e, b=b, dst=dst: e.dma_start(out=ai[b][:], in_=dst), reads=[("moeT", c, s)], writes=["ai%d" % b])
                    for tb in range(2):
                        S.op("dve", lambda e, tb=tb, b=b: e.tensor_tensor(
                            out=ao[b][:, tb * 512:(tb + 1) * 512], in0=pset[tb][:, :], in1=ai[b][:, tb * 512:(tb + 1) * 512], op=ALU.add),
                             reads=[pkey[tb], "ai%d" % b], writes=["ao%d" % b], signal=(tb == 1))
                S.dma("sp", lambda e, b=b, dst=dst: e.dma_start(out=dst, in_=ao[b][:]), reads=["ao%d" % b], writes=[("moeT", c, s)])

        for s in range(TOWN // TS):
            actk = load_resident(S, act, T["x1bT"][:, :, s * TS:(s + 1) * TS], KC, "act")
            gate_up(s, 0, 0)
            for e_ in range(n_experts):
                if e_ + 1 < n_experts:
                    gate_up(s, e_ + 1, (e_ + 1) % 2)
                down(s, e_, e_ % 2, e_ == 0)
        S.end_phase()


ATTN_SCALE = 192.0 ** -0.5


def mla_phase(cx, T, n_heads=32):
    nc, S = cx.nc, cx.S
    tag = "mla"
    with ExitStack() as st:
        sb = lambda n, shp, dt: st.enter_context(nc.sbuf_tensor(tag + n, shp, dt))
        ps = lambda n, shp, dt: st.enter_context(nc.psum_tensor(tag + n, shp, dt))
        cqn = sb("cqn", [128, 8, TOWN], BF16)
        ckvn = sb("ckvn", [128, 4, SEQ], BF16)
        krr = sb("krr", [64, 2, SEQ], BF16)
        cosT = sb("cos", [64, SEQ], F32)
        sinT = sb("sin", [64, SEQ], F32)
        krope = sb("krope", [64, SEQ], BF16)
        ones = sb("ones", [128, 128], BF16)
        ident = sb("ident", [128, 128], BF16)
        qng = sb("qng", [128, 8], F32)
        kvng = sb("kvng", [128, 4], F32)
        sq = [sb("sq%d" % i, [128, 512], BF16) for i in range(2)]
        rb = sb("rb", [128, 512], F32)
        t1 = sb("t1", [64, 1024], F32)
        t2 = sb("t2", [64, 1024], F32)
        kT = sb("kT", [128, SEQ], BF16)
        vaug = sb("vaug", [128, 32, 129], BF16)
        qn = sb("qn", [128, TOWN], BF16)
        qr = sb("qr", [64, TOWN], BF16)
        wq = [sb("wq%d" % i, [128, 8, 256], BF16) for i in range(2)]
        wkv = [sb("wkv%d" % i, [128, 4, 256], BF16) for i in range(2)]
        PT = [sb("PT%d" % i, [128, 512], BF16) for i in range(2)]
        osb = [sb("osb%d" % i, [128, 128], BF16) for i in range(2)]
        rinv = sb("rinv", [128, 512], F32)
        BTh = [sb("BTh%d" % i, [128, TOWN], BF16) for i in range(2)]
        pss = [ps("pss%d" % i, [128, 512], F32) for i in range(2)]
        po = [ps("po%d" % i, [128, 512], F32) for i in range(4)]
        pj = ps("pj", [128, 512], F32)
        pq = ps("pq", [128, 512], F32)
        pqb = pq[:].bitcast(BF16)

        S.dma("sp", lambda e: e.dma_start(out=cqn[:], in_=T["cqT"]), writes=["cqn"])
        S.dma("act", lambda e: e.dma_start(out=ckvn[:], in_=T["ckvT"]), writes=["ckvn"])
        S.dma("sp", lambda e: e.dma_start(out=krr[:], in_=T["krT"]), writes=["krr"])
        S.dma("act", lambda e: e.dma_start(out=cosT[:], in_=T["cosT"]), writes=["cos"])
        S.dma("sp", lambda e: e.dma_start(out=sinT[:], in_=T["sinT"]), writes=["sin"])
        S.dma("sp", lambda e: e.dma_start(out=ident[:], in_=T["ident"]), writes=["ident"])
        S.dma("sp", lambda e: e.dma_start(out=qng[:], in_=T["qng"]), writes=["qng"])
        S.dma("sp", lambda e: e.dma_start(out=kvng[:], in_=T["kvng"]), writes=["kvng"])
        S.op("dve", lambda e: e.memset(ones[:], 1.0), writes=["ones"])
        S.op("dve", lambda e: e.memset(vaug[:, :, 128:129], 1.0), writes=["vaug"])

        def rms(buf, bk, nkc, ntb, gcol, gk, dlat):
            n = 0
            for tb in range(ntb):
                tsl = slice(tb * 512, (tb + 1) * 512)
                for kc in range(nkc):
                    b = n % 2
                    n += 1
                    S.op("pool", lambda e, b=b, kc=kc, tsl=tsl: e.tensor_tensor(out=sq[b][:], in0=buf[:, kc, tsl], in1=buf[:, kc, tsl], op=ALU.mult),
                         reads=[bk], writes=["sq%d" % b])
                    S.op("pe", lambda e, b=b, kc=kc: e.matmul(pj[:, :], lhsT=ones[:], rhs=sq[b][:], start=(kc == 0), stop=(kc == nkc - 1)),
                         reads=["sq%d" % b, "ones"], writes=["pj"])
                S.op("dve", lambda e: e.tensor_scalar(out=rb[:], in0=pj[:, :], scalar1=1.0 / dlat, scalar2=RMS_EPS, op0=ALU.mult, op1=ALU.add),
                     reads=["pj"], writes=["rb"])
                S.op("act", lambda e: e.activation(out=rb[:], in_=rb[:], func=AF.Sqrt), reads=["rb"], writes=["rb"])
                S.op("dve", lambda e: e.reciprocal(out=rb[:], in_=rb[:]), reads=["rb"], writes=["rb"])
                for kc in range(nkc):
                    S.op("dve", lambda e, kc=kc, tsl=tsl: e.scalar_tensor_tensor(
                        out=buf[:, kc, tsl], in0=buf[:, kc, tsl], scalar=gcol[:, kc:kc + 1], in1=rb[:], op0=ALU.mult, op1=ALU.mult),
                         reads=[bk, gk, "rb"], writes=[bk])

        rms(cqn, "cqn", 8, 4, qng, "qng", 1024.0)
        rms(ckvn, "ckvn", 4, 8, kvng, "kvng", 512.0)
        for blk in range(4):
            tsl = slice(blk * 1024, (blk + 1) * 1024)
            S.op("dve", lambda e, tsl=tsl: e.tensor_tensor(out=t1[:], in0=krr[:, 0, tsl], in1=cosT[:, tsl], op=ALU.mult),
                 reads=["krr", "cos"], writes=["t1"])
            S.op("pool", lambda e, tsl=tsl: e.tensor_tensor(out=t2[:], in0=krr[:, 1, tsl], in1=sinT[:, tsl], op=ALU.mult),
                 reads=["krr", "sin"], writes=["t2"])
            S.op("dve", lambda e, tsl=tsl: e.tensor_tensor(out=krope[:, tsl], in0=t1[:], in1=t2[:], op=ALU.add),
                 reads=["t1", "t2"], writes=["krope"])

        flip = [0]

        def evac(out_ap, in_ap, reads, writes):
            flip[0] ^= 1
            if flip[0]:
                S.op("act", lambda e: e.activation(out=out_ap, in_=in_ap, func=AF.Copy), reads=reads, writes=writes)
            else:
                S.op("dve", lambda e: e.tensor_copy(out=out_ap, in_=in_ap), reads=reads, writes=writes)

        for h in range(n_heads):
            ws = h % 2
            wqt, wkvt = wq[ws], wkv[ws]
            wqk, wkvk = "wq%d" % ws, "wkv%d" % ws
            S.dma("pool", lambda e, wqt=wqt, h=h: e.dma_start(out=wqt[:], in_=T["wuq_h"][h], max_dma_last_dim=4096), writes=[wqk])
            S.dma("pool", lambda e, wkvt=wkvt, h=h: e.dma_start(out=wkvt[:], in_=T["wukv_h"][h], max_dma_last_dim=4096), writes=[wkvk])
            for tb in range(8):
                tsl = slice(tb * 512, (tb + 1) * 512)
                pb, pbk = (pj, "pj") if tb % 2 == 0 else (pq, "pq")
                for kc in range(4):
                    S.op("pe", lambda e, kc=kc, tsl=tsl, pb=pb, wkvt=wkvt: e.matmul(pb[:, :], lhsT=wkvt[:, kc, 0:128], rhs=ckvn[:, kc, tsl],
                                                                    start=(kc == 0), stop=(kc == 3)),
                         reads=[wkvk, "ckvn"], writes=[pbk], signal=(kc == 3))
                evac(kT[:, tsl], pb[:, :], [pbk], ["kT"])
            for g in range(8):
                pb, pbk = (pj, "pj") if g % 2 == 0 else (pq, "pq")
                for j in range(4):
                    tt = g * 4 + j
                    for kc in range(4):
                        S.op("pe", lambda e, kc=kc, tt=tt, j=j, pb=pb, wkvt=wkvt: e.matmul(
                            pb[:, j * 128:(j + 1) * 128], lhsT=ckvn[:, kc, tt * 128:(tt + 1) * 128], rhs=wkvt[:, kc, 128:256],
                            start=(kc == 0), stop=(kc == 3)),
                             reads=[wkvk, "ckvn"], writes=[pbk], signal=(kc == 3 and j == 3))
                evac(vaug[:, g * 4:(g + 1) * 4, 0:128], pb[:, :].rearrange("p (j c) -> p j c", c=128), [pbk], ["vaug"])
            for tb in range(4):
                tsl = slice(tb * 512, (tb + 1) * 512)
                for kc in range(8):
                    S.op("pe", lambda e, kc=kc, tsl=tsl, wqt=wqt: e.matmul(pj[:, :], lhsT=wqt[:, kc, 0:128], rhs=cqn[:, kc, tsl],
                                                                  start=(kc == 0), stop=(kc == 7)),
                         reads=[wqk, "cqn"], writes=["pj"], signal=(kc == 7))
                evac(qn[:, tsl], pj[:, :], ["pj"], ["qn"])
                for kc in range(8):
                    S.op("pe", lambda e, kc=kc, tsl=tsl, wqt=wqt: e.matmul(pj[0:64, :], lhsT=wqt[:, kc, 128:192], rhs=cqn[:, kc, tsl],
                                                                  start=(kc == 0), stop=(kc == 7)),
                         reads=[wqk, "cqn"], writes=["pj"], signal=(kc == 7))
                for kc in range(8):
                    S.op("pe", lambda e, kc=kc, tsl=tsl, wqt=wqt: e.matmul(pq[0:64, :], lhsT=wqt[:, kc, 192:256], rhs=cqn[:, kc, tsl],
                                                                  start=(kc == 0), stop=(kc == 7)),
                         reads=[wqk, "cqn"], writes=["pq"], signal=(kc == 7))
                S.op("dve", lambda e, tsl=tsl: e.tensor_tensor(out=t1[:, 0:512], in0=pj[0:64, :], in1=cosT[:, tsl], op=ALU.mult),
                     reads=["pj", "cos"], writes=["t1"])
                S.op("dve", lambda e, tsl=tsl: e.tensor_tensor(out=t2[:, 0:512], in0=pq[0:64, :], in1=sinT[:, tsl], op=ALU.mult),
                     reads=["pq", "sin"], writes=["t2"])
                S.op("pool", lambda e, tsl=tsl: e.tensor_tensor(out=qr[:, tsl], in0=t1[:, 0:512], in1=t2[:, 0:512], op=ALU.add),
                     reads=["t1", "t2"], writes=["qr"])
            bsl = h % 2
            for qb in range(4):
                qsl = slice(qb * 512, (qb + 1) * 512)
                pa, pak = po[(qb % 2) * 2], "po%d" % ((qb % 2) * 2)
                pbb, pbbk = po[(qb % 2) * 2 + 1], "po%d" % ((qb % 2) * 2 + 1)

                def score(kt, qsl=qsl):
                    ksl = slice(kt * 128, (kt + 1) * 128)
                    pb, pbk = pss[kt % 2], "pss%d" % (kt % 2)
                    S.op("pe", lambda e: e.matmul(pb[:, :], lhsT=kT[:, ksl], rhs=qn[:, qsl], start=True, stop=False),
                         reads=["kT", "qn"], writes=[pbk], signal=False)
                    S.op("pe", lambda e: e.matmul(pb[:, :], lhsT=krope[:, ksl], rhs=qr[:, qsl], start=False, stop=True),
                         reads=["krope", "qr"], writes=[pbk])

                score(0)
                for kt in range(32):
                    if kt + 1 < 32:
                        score(kt + 1)
                    pb, pbk = pss[kt % 2], "pss%d" % (kt % 2)
                    pt, ptk = PT[kt % 2], "PT%d" % (kt % 2)
                    S.op("act", lambda e, pb=pb, pt=pt: e.activation(out=pt[:], in_=pb[:, :], func=AF.Exp, scale=ATTN_SCALE),
                         reads=[pbk], writes=[ptk])
                    S.op("pe", lambda e, pt=pt, kt=kt, pa=pa: e.matmul(pa[:, :], lhsT=vaug[:, kt, 0:128], rhs=pt[:],
                                                                start=(kt == 0), stop=(kt == 31)),
                         reads=[ptk, "vaug"], writes=[pak], signal=False)
                    S.op("pe", lambda e, pt=pt, kt=kt, pbb=pbb: e.matmul(pbb[:, :], lhsT=ones[:], rhs=pt[:],
                                                                  start=(kt == 0), stop=(kt == 31)),
                         reads=[ptk, "ones"], writes=[pbbk])
                S.op("dve", lambda e, pbb=pbb: e.reciprocal(out=rinv[:], in_=pbb[:, :]), reads=[pbbk], writes=["rinv"])
                S.op("dve", lambda e, pa=pa, qsl=qsl, bsl=bsl: e.tensor_tensor(out=BTh[bsl][:, qsl], in0=pa[:, :], in1=rinv[:], op=ALU.mult),
                     reads=[pak, "rinv"], writes=["BTh%d" % bsl])
            S.dma("sp", lambda e, h=h, bsl=bsl: e.dma_start(out=T["BT"][:, h, :], in_=BTh[bsl][:]),
                  reads=["BTh%d" % bsl], writes=[("BT", h)])
        S.end_phase()


def mlstm_phase(cx, T, n_own=32, n_all=64):
    nc, S = cx.nc, cx.S
    tag = "ml"
    NCH = n_all
    with ExitStack() as st:
        sb = lambda n, shp, dt: st.enter_context(nc.sbuf_tensor(tag + n, shp, dt))
        ps = lambda n, shp, dt: st.enter_context(nc.psum_tensor(tag + n, shp, dt))
        G = sb("G", [64, NCH, 32], F32)
        tri = sb("tri", [64, 4, 64], F32)
        maskb = sb("maskb", [64, 2, 64], F32)
        ones64 = sb("ones64", [64, 128], F32)
        ones64b = sb("ones64b", [64, 2], BF16)
        ident = sb("ident", [128, 128], BF16)
        hng = sb("hng", [128, 32], F32)
        lf = sb("lf", [64, NCH * 8], F32)
        tmpg = sb("tmpg", [64, NCH * 8], F32)
        u = [sb("u%d" % d, [64, NCH * 8], F32) for d in range(2)]
        fl = [sb("fl%d" % d, [64, NCH * 8], F32) for d in range(2)]
        wsx = [sb("ws%d" % d, [64, NCH * 8], F32) for d in range(2)]
        dec = [sb("dec%d" % d, [128, NCH * 8], F32) for d in range(2)]
        C = sb("C", [128, 8, 2, 512], F32)
        Cb = sb("Cb", [128, 8, 2, 512], BF16)
        nst = sb("nst", [128, 8, 2], F32)
        nb = sb("nb", [128, 8, 2], BF16)
        qTc = [sb("qTc%d" % i, [128, 16, 64], BF16) for i in range(2)]
        kTc = [sb("kTc%d" % i, [128, 16, 64], BF16) for i in range(2)]
        vch = [sb("vch%d" % i, [64, 4096], BF16) for i in range(2)]
        soch = sb("soch", [64, 4096], BF16)
        hfin = sb("hfin", [64, 4096], F32)
        hsum = sb("hsum", [64, 4096], F32)
        kws = [sb("kws%d" % i, [64, 8, 256], BF16) for i in range(2)]
        stf = sb("stf", [64, 8, 64], F32)
        sTm = [sb("sTm%d" % i, [64, 8, 64], BF16) for i in range(2)]
        rden = sb("rden", [64, 8], F32)
        ss = sb("ss", [64, 8], F32)
        sqj = sb("sqj", [64, 512], F32)
        Abf = sb("Abf", [64, 4096], BF16)
        ATs = sb("ATs", [128, 32, 256], BF16)
        pst = ps("pst", [128, 512], F32)
        ptr_ = ps("ptr", [128, 1024], F32)
        ptrb = ptr_[:].bitcast(BF16)
        pn = [ps("pn%d" % i, [128, 512], F32) for i in range(2)]
        pu = [ps("pu%d" % i, [128, 512], F32) for i in range(2)]
        pd = ps("pd", [128, 512], F32)

        S.dma("sp", lambda e: e.dma_start(out=G[:], in_=T["gtok"].rearrange("(c p) g -> p c g", p=64)), writes=["G"])
        S.dma("sp", lambda e: e.dma_start(out=tri[:], in_=T["tri"]), writes=["tri"])
        S.dma("sp", lambda e: e.dma_start(out=ident[:], in_=T["ident"]), writes=["ident"])
        S.dma("sp", lambda e: e.dma_start(out=hng[:], in_=T["hng"]), writes=["hng"])
        S.op("dve", lambda e: e.memset(ones64[:], 1.0), writes=["ones64"])
        S.op("dve", lambda e: e.memset(ones64b[:], 1.0), writes=["ones64b"])
        S.op("dve", lambda e: e.tensor_copy(out=maskb[:], in_=tri[:, 0:2, :]), reads=["tri"], writes=["maskb"])

        NG = NCH * 8
        v3 = lambda t: t[:].rearrange("p (c h) -> p c h", h=8)
        for d in range(2):
            iv = G[:, :, 16 * d:16 * d + 8]
            fv = G[:, :, 16 * d + 8:16 * d + 16]
            S.op("act", lambda e, fv=fv: e.activation(out=v3(lf), in_=fv, func=AF.Sigmoid), reads=["G"], writes=["lf"])
            S.op("act", lambda e: e.activation(out=lf[:], in_=lf[:], func=AF.Ln), reads=["lf"], writes=["lf"])
            banks = [pst, pn[0], pn[1]]
            bkeys = ["pst", "pn0", "pn1"]
            lhs = [tri[:, d, :], tri[:, 2 + d, :], ones64[:]]
            for i in range(3):
                M = 128 if i == 2 else 64
                S.op("pe", lambda e, i=i, M=M, banks=banks, lhs=lhs: e.matmul(banks[i][:M, 0:NG], lhsT=lhs[i], rhs=lf[:], start=True, stop=True),
                     reads=["lf", "tri", "ones64"], writes=[bkeys[i]])
            S.op("dve", lambda e, iv=iv: e.tensor_tensor(out=v3(tmpg), in0=iv, in1=pst[0:64, 0:NG].rearrange("p (c h) -> p c h", h=8), op=ALU.subtract),
                 reads=["G", "pst"], writes=["tmpg"])
            S.op("act", lambda e, d=d: e.activation(out=u[d][:], in_=tmpg[:], func=AF.Exp), reads=["tmpg"], writes=["u%d" % d])
            S.op("dve", lambda e, d=d: e.tensor_scalar(out=u[d][:], in0=u[d][:], scalar1=0.0625, scalar2=None, op0=ALU.mult),
                 reads=["u%d" % d], writes=["u%d" % d])
            S.op("act", lambda e, d=d: e.activation(out=fl[d][:], in_=pst[0:64, 0:NG], func=AF.Exp, scale=-1.0),
                 reads=["pst"], writes=["fl%d" % d])
            S.op("dve", lambda e, iv=iv: e.tensor_tensor(out=v3(tmpg), in0=iv, in1=pn[0][0:64, 0:NG].rearrange("p (c h) -> p c h", h=8), op=ALU.add),
                 reads=["G", "pn0"], writes=["tmpg"])
            S.op("act", lambda e, d=d: e.activation(out=wsx[d][:], in_=tmpg[:], func=AF.Exp), reads=["tmpg"], writes=["ws%d" % d])
            S.op("dve", lambda e, d=d: e.tensor_scalar(out=wsx[d][:], in0=wsx[d][:], scalar1=0.0625, scalar2=None, op0=ALU.mult),
                 reads=["ws%d" % d], writes=["ws%d" % d])
            S.op("act", lambda e, d=d: e.activation(out=dec[d][:], in_=pn[1][:, 0:NG], func=AF.Exp),
                 reads=["pn1"], writes=["dec%d" % d])

        visit = [0]

        def chunk(c, d, full):
            vi = visit[0]
            visit[0] += 1
            sl = vi % 2
            kt_, kk = kTc[sl], "kTc%d" % sl
            qt_, qk = qTc[sl], "qTc%d" % sl
            vt_, vk = vch[sl], "vch%d" % sl
            kw_, kwk = kws[sl], "kws%d" % sl
            sm_, smk = sTm[sl], "sTm%d" % sl
            tsl = slice(c * 64, (c + 1) * 64)
            last_out = d == 1
            S.dma("sp", lambda e: e.dma_start(out=kt_[:], in_=T["kcT"][:, :, tsl]), writes=[kk])
            S.dma("sp", lambda e: e.dma_start(out=vt_[:], in_=T["vtok"][tsl, :]), writes=[vk])
            if full:
                S.dma("sp", lambda e: e.dma_start(out=qt_[:], in_=T["qcT"][:, :, tsl]), writes=[qk])
                if last_out:
                    S.dma("sp", lambda e: e.dma_start(out=soch[:], in_=T["sotok"][tsl, :]), writes=["soch"])
                    S.dma("sp", lambda e: e.dma_start(out=hfin[:], in_=T["hfwd"][tsl, :]), reads=[("hfwd", c)], writes=["hfin"])
            for i in range(16):
                S.op("pe", lambda e, i=i: e.transpose(ptrb[0:64, i * 128:(i + 1) * 128], kt_[:, i, :], ident[:]),
                     reads=[kk, "ident"], writes=["ptr"], signal=(i == 15))
            S.op("dve", lambda e: e.tensor_tensor(
                out=kw_[:], in0=ptrb[0:64, :].rearrange("p (h x) -> p h x", x=256),
                in1=wsx[d][:, c * 8:(c + 1) * 8].unsqueeze(2).to_broadcast([64, 8, 256]), op=ALU.mult),
                 reads=["ptr", "ws%d" % d], writes=[kwk])
            if full:
                for h in range(8):
                    for j in range(2):
                        S.op("pe", lambda e, h=h, j=j: e.matmul(pst[0:64, h * 64:(h + 1) * 64], lhsT=kt_[:, 2 * h + j, :], rhs=qt_[:, 2 * h + j, :],
                                                                start=(j == 0), stop=(j == 1)),
                             reads=[kk, qk], writes=["pst"], signal=(h == 7 and j == 1))
                S.op("dve", lambda e: e.tensor_tensor(
                    out=stf[:], in0=pst[0:64, :].rearrange("p (h t) -> p h t", t=64),
                    in1=u[d][:, c * 8:(c + 1) * 8].unsqueeze(2).to_broadcast([64, 8, 64]), op=ALU.mult),
                     reads=["pst", "u%d" % d], writes=["stf"])
                S.op("pool", lambda e: e.tensor_tensor(
                    out=sm_[:], in0=stf[:], in1=maskb[:, d, :].unsqueeze(1).to_broadcast([64, 8, 64]), op=ALU.mult),
                     reads=["stf", "maskb"], writes=[smk])
                for h in range(8):
                    for j in range(2):
                        S.op("pe", lambda e, h=h, j=j: e.matmul(pd[0:64, h:h + 1], lhsT=qt_[:, 2 * h + j, :], rhs=nb[:, h, j:j + 1],
                                                                start=(j == 0), stop=False),
                             reads=[qk, "nb"], writes=["pd"], signal=False)
                    S.op("pe", lambda e, h=h: e.matmul(pd[0:64, h:h + 1], lhsT=sm_[:, h, :], rhs=ones64b[:, 0:1], start=False, stop=True),
                         reads=[smk, "ones64b"], writes=["pd"], signal=(h == 7))
                S.op("dve", lambda e: e.tensor_scalar(out=rden[:], in0=pd[0:64, 0:8], scalar1=-1.0, scalar2=None, op0=ALU.mult),
                     reads=["pd"], writes=["rden"])
                S.op("dve", lambda e: e.tensor_tensor(out=rden[:], in0=rden[:], in1=pd[0:64, 0:8], op=ALU.max),
                     reads=["pd", "rden"], writes=["rden"])
                S.op("dve", lambda e: e.tensor_tensor(out=rden[:], in0=rden[:], in1=fl[d][:, c * 8:(c + 1) * 8], op=ALU.max),
                     reads=["rden", "fl%d" % d], writes=["rden"])
                S.op("dve", lambda e: e.reciprocal(out=rden[:], in_=rden[:]), reads=["rden"], writes=["rden"])
                for h in range(8):
                    pb, pbk = pn[h % 2], "pn%d" % (h % 2)
                    hs = slice(h * 512, (h + 1) * 512)
                    for j in range(2):
                        S.op("pe", lambda e, h=h, j=j, pb=pb: e.matmul(pb[0:64, :], lhsT=qt_[:, 2 * h + j, :], rhs=Cb[:, h, j, :],
                                                                       start=(j == 0), stop=False),
                             reads=[qk, "Cb"], writes=[pbk], signal=False)
                    S.op("pe", lambda e, h=h, pb=pb, hs=hs: e.matmul(pb[0:64, :], lhsT=sm_[:, h, :], rhs=vt_[:, hs], start=False, stop=True),
                         reads=[smk, vk], writes=[pbk])
                    if d == 0:
                        S.op("act", lambda e, h=h, pb=pb, hs=hs: e.activation(out=hsum[:, hs], in_=pb[0:64, :], func=AF.Copy, scale=rden[:, h:h + 1]),
                             reads=[pbk, "rden"], writes=["hsum"])
                    else:
                        S.op("dve", lambda e, h=h, pb=pb, hs=hs: e.scalar_tensor_tensor(
                            out=hsum[:, hs], in0=pb[0:64, :], scalar=rden[:, h:h + 1], in1=hfin[:, hs], op0=ALU.mult, op1=ALU.add),
                             reads=[pbk, "rden", "hfin"], writes=["hsum"])
            for h in range(8):
                hs = slice(h * 512, (h + 1) * 512)
                dcol = dec[d][:, c * 8 + h:c * 8 + h + 1]
                for j in range(2):
                    S.op("pe", lambda e, h=h, j=j, hs=hs: e.matmul(pu[j][:, :], lhsT=kw_[:, h, j * 128:(j + 1) * 128], rhs=vt_[:, hs], start=True, stop=True),
                         reads=[kwk, vk], writes=["pu%d" % j])
                    S.op("pe", lambda e, h=h, j=j: e.matmul(pd[:, 8 + 2 * h + j:9 + 2 * h + j], lhsT=kw_[:, h, j * 128:(j + 1) * 128], rhs=ones64b[:, 0:1],
                                                            start=True, stop=True),
                         reads=[kwk, "ones64b"], writes=["pd"])
                for j in range(2):
                    S.op("dve", lambda e, h=h, j=j, dcol=dcol: e.scalar_tensor_tensor(
                        out=C[:, h, j, :], in0=C[:, h, j, :], scalar=dcol, in1=pu[j][:, :], op0=ALU.mult, op1=ALU.add),
                         reads=["C", "pu%d" % j, "dec%d" % d], writes=["C"])
                S.op("act", lambda e, h=h: e.activation(out=Cb[:, h, :, :], in_=C[:, h, :, :], func=AF.Copy), reads=["C"], writes=["Cb"])
                S.op("dve", lambda e, h=h, dcol=dcol: e.scalar_tensor_tensor(
                    out=nst[:, h, :], in0=nst[:, h, :], scalar=dcol, in1=pd[:, 8 + 2 * h:10 + 2 * h], op0=ALU.mult, op1=ALU.add),
                     reads=["nst", "pd", "dec%d" % d], writes=["nst"])
                S.op("dve", lambda e, h=h: e.tensor_copy(out=nb[:, h, :], in_=nst[:, h, :]), reads=["nst"], writes=["nb"])
            if full and d == 0:
                S.dma("pool", lambda e: e.dma_start(out=T["hfwd"][tsl, :], in_=hsum[:]), reads=["hsum"], writes=[("hfwd", c)])
            if full and d == 1:
                for h in range(8):
                    hs = slice(h * 512, (h + 1) * 512)
                    S.op("act", lambda e, h=h, hs=hs: e.activation(out=sqj[:], in_=hsum[:, hs], func=AF.Square, accum_out=ss[:, h:h + 1]),
                         reads=["hsum"], writes=["sqj", "ss"])
                S.op("dve", lambda e: e.tensor_scalar(out=ss[:], in0=ss[:], scalar1=1.0 / 512, scalar2=RMS_EPS, op0=ALU.mult, op1=ALU.add),
                     reads=["ss"], writes=["ss"])
                S.op("act", lambda e: e.activation(out=ss[:], in_=ss[:], func=AF.Sqrt), reads=["ss"], writes=["ss"])
                S.op("dve", lambda e: e.reciprocal(out=ss[:], in_=ss[:]), reads=["ss"], writes=["ss"])
                for h in range(8):
                    hs = slice(h * 512, (h + 1) * 512)
                    S.op("dve", lambda e, h=h, hs=hs: e.scalar_tensor_tensor(
                        out=Abf[:, hs], in0=hsum[:, hs], scalar=ss[:, h:h + 1], in1=soch[:, hs], op0=ALU.mult, op1=ALU.mult),
                         reads=["hsum", "ss", "soch"], writes=["Abf"])
                for i in range(32):
                    S.op("pe", lambda e, i=i: e.transpose(ptrb[:, i * 64:(i + 1) * 64], Abf[:, i * 128:(i + 1) * 128], ident[0:64, 0:64]),
                         reads=["Abf", "ident"], writes=["ptr"], signal=(i == 31))
                q4 = c % 4
                S.op("dve", lambda e, q4=q4: e.tensor_tensor(
                    out=ATs[:, :, q4 * 64:(q4 + 1) * 64], in0=ptrb[:, :].rearrange("p (k t) -> p k t", t=64),
                    in1=hng[:].unsqueeze(2).to_broadcast([128, 32, 64]), op=ALU.mult),
                     reads=["ptr", "hng"], writes=["ATs"])
                if q4 == 0:
                    S.dma("pool", lambda e: e.dma_start(out=T["AT"][:, :, c * 64:c * 64 + 256], in_=ATs[:]),
                          reads=["ATs"], writes=[("AT", c)])

        def reset_state():
            S.op("dve", lambda e: e.memset(C[:], 0.0), writes=["C"])
            S.op("pool", lambda e: e.memset(Cb[:], 0.0), writes=["Cb"])
            S.op("dve", lambda e: e.memset(nst[:], 0.0), writes=["nst"])
            S.op("dve", lambda e: e.memset(nb[:], 0.0), writes=["nb"])

        reset_state()
        for c in range(n_own):
            chunk(c, 0, True)
        reset_state()
        for c in range(NCH - 1, n_own - 1, -1):
            chunk(c, 1, False)
        for c in range(n_own - 1, -1, -1):
            chunk(c, 1, True)
        S.end_phase()


def build_program(upto="all", moe_experts=32, mla_heads=32):
    nc = bass.Bass("TRN2", target_bir_lowering=False)
    stack = ExitStack()
    S = Sched(nc, stack)
    cx = Ctx(nc, S)
    T = {}

    def inp(name, shape, dt=F32):
        T[name] = nc.dram_tensor(name, list(shape), dt, kind="ExternalInput").ap()
        return T[name]

    def scr(name, shape, dt):
        T[name] = cx.scratch(name, shape, dt)
        return T[name]

    xT = inp("xT", [128, KC, SEQ])
    ln0g, ln0b = inp("ln0_g", [128, KC]), inp("ln0_b", [128, KC])
    inp("ident", [128, 128], BF16)
    inp("identf", [128, 128])
    inp("win_ch", [len(P1_CHUNKS), 128, KC, 128])
    inp("wg", [128, KC, 32])
    inp("bg", [128, 32])
    inp("convw", [128, 32, 3])
    inp("tri", [64, 4, 64])
    inp("hng", [128, 32])
    inp("wuq_h", [32, 128, 8, 256])
    inp("wukv_h", [32, 128, 4, 256])
    inp("qng", [128, 8])
    inp("kvng", [128, 4])
    inp("cosT", [64, SEQ])
    inp("sinT", [64, SEQ])
    for n in ("wpm_ch", "wpa_ch", "wout_ch"):
        inp(n, [KC, 128, KC, 128])
    for n in ("ln1_g", "ln1_b", "ln2_g", "ln2_b"):
        inp(n, [128, KC])
    inp("wr", [128, KC, 40])
    inp("br", [128, 40])
    inp("wge_ch", [32, 8, 128, KC, 128])
    inp("wue_ch", [32, 8, 128, KC, 128])
    inp("wde_ch", [32, KC, 128, 8, 128])

    xnT = scr("xnT", [128, KC, SEQ], BF16)
    x0T = scr("x0T", [128, KC, TOWN], F32)
    scr("qcT", [128, 16, TOWN], BF16)
    scr("kcT", [128, 16, SEQ], BF16)
    scr("vtok", [SEQ, 4096], BF16)
    scr("sotok", [TOWN, 4096], BF16)
    scr("gtok", [SEQ, 32], F32)
    scr("cqT", [128, 8, TOWN], BF16)
    scr("ckvT", [128, 4, SEQ], BF16)
    scr("krT", [64, 2, SEQ], BF16)
    scr("sgaT", [128, KC, TOWN], BF16)
    scr("sgbT", [128, KC, TOWN], BF16)
    scr("hfwd", [TOWN, 4096], F32)
    scr("AT", [128, KC, TOWN], BF16)
    scr("BT", [128, KC, TOWN], BF16)
    scr("mixaT", [128, KC, TOWN], F32)
    scr("mixT", [128, KC, TOWN], BF16)
    scr("z1T", [128, KC, TOWN], F32)
    scr("x1T", [128, KC, TOWN], F32)
    scr("x1bT", [128, KC, TOWN], BF16)
    scr("combT", [32, TOWN], F32)
    scr("moeT", [128, KC, TOWN], F32)
    T["outT"] = nc.dram_tensor("outT", [128, KC, TOWN], F32, kind="ExternalOutput").ap()

    Tb = 256
    with ExitStack() as st0:
        ybf = [st0.enter_context(nc.sbuf_tensor("p0ybf%d" % i, [128, KC, Tb], BF16)) for i in range(2)]

        def load_z(blk, zt, zk):
            S.dma("sp", lambda e: e.dma_start(out=zt[:], in_=xT[:, :, blk * Tb:(blk + 1) * Tb]), writes=[zk])

        def store_y(blk, yt, yk, st):
            sl = blk % 2
            S.op("pool", lambda e: e.tensor_copy(out=ybf[sl][:], in_=yt[:]), reads=[yk], writes=["ybf%d" % sl])
            S.dma("act", lambda e: e.dma_start(out=xnT[:, :, blk * Tb:(blk + 1) * Tb], in_=ybf[sl][:]),
                  reads=["ybf%d" % sl], writes=[("xnT", blk)])
            if (blk + 1) * Tb <= TOWN:
                S.dma("act", lambda e: e.dma_start(out=x0T[:, :, blk * Tb:(blk + 1) * Tb], in_=yt[:]),
                      reads=[yk], writes=[("x0T", blk)])

        ln_phase(cx, "p0", SEQ, Tb, load_z, ln0g, ln0b, store_y)

    stages = ["p0", "p1", "ml", "mla", "p4", "ln1", "rt", "moe", "all"]
    lvl = stages.index(upto)
    if lvl >= 1:
        p1_pass(cx, T, False)
        p1_pass(cx, T, True)
    if lvl >= 2:
        mlstm_phase(cx, T)
    if lvl >= 3:
        mla_phase(cx, T, mla_heads)
    if lvl >= 4:
        p4_phase(cx, T, "a")
        p4_phase(cx, T, "b")
        p4_phase(cx, T, "c")
    if lvl >= 5:
        with ExitStack() as st1:
            ybf = [st1.enter_context(nc.sbuf_tensor("l1ybf%d" % i, [128, KC, Tb], BF16)) for i in range(2)]

            def load_z1(blk, zt, zk):
                S.dma("sp", lambda e: e.dma_start(out=zt[:], in_=T["z1T"][:, :, blk * Tb:(blk + 1) * Tb]), writes=[zk])

            def store_y1(blk, yt, yk, st):
                sl = blk % 2
                S.op("pool", lambda e: e.tensor_copy(out=ybf[sl][:], in_=yt[:]), reads=[yk], writes=["ybf%d" % sl])
                S.dma("act", lambda e: e.dma_start(out=T["x1bT"][:, :, blk * Tb:(blk + 1) * Tb], in_=ybf[sl][:]),
                      reads=["ybf%d" % sl], writes=[("x1bT", blk)])
                S.dma("act", lambda e: e.dma_start(out=T["x1T"][:, :, blk * Tb:(blk + 1) * Tb], in_=yt[:]),
                      reads=[yk], writes=[("x1T", blk)])

            ln_phase(cx, "l1", TOWN, Tb, load_z1, T["ln1_g"], T["ln1_b"], store_y1)
    if lvl >= 6:
        router_phase(cx, T)
    if lvl >= 7:
        moe_phase(cx, T, moe_experts)
    if lvl >= 8:
        with ExitStack() as st2:
            mo = [st2.enter_context(nc.sbuf_tensor("l2mo%d" % i, [128, KC, Tb], F32)) for i in range(1)]

            def load_z2(blk, zt, zk):
                sl = 0
                S.dma("sp", lambda e: e.dma_start(out=zt[:], in_=T["x1T"][:, :, blk * Tb:(blk + 1) * Tb]), writes=[zk])
                S.dma("act", lambda e: e.dma_start(out=mo[sl][:], in_=T["moeT"][:, :, blk * Tb:(blk + 1) * Tb]), writes=["mo%d" % sl])
                S.op("dve", lambda e: e.scalar_tensor_tensor(out=zt[:], in0=zt[:], scalar=ALPHA, in1=mo[sl][:], op0=ALU.mult, op1=ALU.add),
                     reads=[zk, "mo%d" % sl], writes=[zk])

            def store_y2(blk, yt, yk, st):
                S.dma("act", lambda e: e.dma_start(out=T["outT"][:, :, blk * Tb:(blk + 1) * Tb], in_=yt[:]),
                      reads=[yk], writes=[("outT", blk)])

            ln_phase(cx, "l2", TOWN, Tb, load_z2, T["ln2_g"], T["ln2_b"], store_y2)

    stack.close()
    return nc


def fm(a2d):
    t, f = a2d.shape
    return np.ascontiguousarray(a2d.T.reshape(f // 128, 128, t).transpose(1, 0, 2))


def col128(v):
    return np.ascontiguousarray(v.reshape(-1, 128).T)


def wtile(w, cols):
    k = w.shape[0]
    out = np.zeros((128, k // 128, 128), np.float32)
    out[:, :, :len(cols)] = w[:, cols].reshape(k // 128, 128, len(cols)).transpose(1, 0, 2)
    return out


def wchunks(w):
    k, n = w.shape
    return np.ascontiguousarray(w.reshape(k // 128, 128, n // 128, 128).transpose(2, 1, 0, 3))


def shared_maps(inputs, stage="all"):
    import ml_dtypes
    f = lambda k: np.asarray(inputs[k], np.float32)
    w_in = f("w_in")[0]
    base = {"ln0_g": col128(f("ln0_g")), "ln0_b": col128(f("ln0_b")),
            "ident": np.eye(128, dtype=np.float32).astype(ml_dtypes.bfloat16),
            "identf": np.eye(128, dtype=np.float32)}
    wch = np.zeros((len(P1_CHUNKS), 128, KC, 128), np.float32)
    for n, (fam, fi, M) in enumerate(P1_CHUNKS):
        if fam == "kr":
            cols = W_OFF["kr"] + (np.arange(64) if fi == 0 else np.concatenate([np.arange(32, 64), np.arange(0, 32)]))
        else:
            cols = W_OFF[fam] + fi * 128 + np.arange(128)
        wch[n] = wtile(w_in, cols)
    base["win_ch"] = wch
    s_, t_ = np.meshgrid(np.arange(64), np.arange(64), indexing="ij")
    base["tri"] = np.ascontiguousarray(np.stack([s_ <= t_, s_ >= t_, s_ > t_, s_ < t_], 1).astype(np.float32))
    base["hng"] = col128(f("head_norm_g")[0])
    w_uq = f("w_uq")[0]
    wq = np.zeros((32, 128, 8, 256), np.float32)
    perm = np.concatenate([np.arange(32, 64), np.arange(0, 32)])
    for h in range(32):
        cols = np.concatenate([h * 192 + np.arange(192), h * 192 + 128 + perm])
        wq[h] = w_uq[:, cols].reshape(8, 128, 256).transpose(1, 0, 2)
    base["wuq_h"] = wq
    w_ukv = f("w_ukv")[0]
    base["wukv_h"] = np.ascontiguousarray(w_ukv.reshape(4, 128, 32, 256).transpose(2, 1, 0, 3))
    base["qng"] = col128(f("q_norm_g")[0])
    base["kvng"] = col128(f("kv_norm_g")[0])
    base["wpm_ch"] = wchunks(f("w_pm")[0])
    base["wpa_ch"] = wchunks(f("w_pa")[0])
    base["wout_ch"] = wchunks(f("w_out")[0])
    for n in ("ln1_g", "ln1_b", "ln2_g", "ln2_b"):
        base[n] = col128(f(n)[0])
    wr = np.concatenate([f("w_rg")[0], f("w_re")[0]], 1)
    base["wr"] = np.ascontiguousarray(wr.reshape(32, 128, 40).transpose(1, 0, 2))
    base["br"] = np.ascontiguousarray(np.broadcast_to(np.concatenate([f("b_rg")[0], f("b_re")[0]])[None, :], (128, 40)))
    base["wge_ch"] = np.ascontiguousarray(f("w_e_gate")[0].reshape(32, 32, 128, 8, 128).transpose(0, 3, 2, 1, 4))
    base["wue_ch"] = np.ascontiguousarray(f("w_e_up")[0].reshape(32, 32, 128, 8, 128).transpose(0, 3, 2, 1, 4))
    base["wde_ch"] = np.ascontiguousarray(f("w_e_down")[0].reshape(32, 8, 128, 32, 128).transpose(0, 3, 2, 1, 4))
    inv = 1.0 / (10000.0 ** (np.arange(0, 64, 2, dtype=np.float32) / 64.0))
    ang = np.arange(SEQ, dtype=np.float32)[:, None] * inv[None, :]
    cos, sin = np.cos(ang).astype(np.float32), np.sin(ang).astype(np.float32)
    cosT = np.concatenate([cos, cos], 1).T
    sinT = np.concatenate([-sin, sin], 1).T
    out = []
    for hf in range(2):
        m = dict(base)
        gperm = np.arange(32) if hf == 0 else np.concatenate([np.arange(16, 32), np.arange(0, 16)])
        m["wg"] = np.ascontiguousarray(wtile(w_in, W_OFF["g"] + gperm)[:, :, :32])
        m["bg"] = np.ascontiguousarray(np.broadcast_to(f("b_gates")[0][gperm][None, :], (128, 32)))
        cw = f("conv_qk")[0]
        if hf == 1:
            cw = cw[::-1]
        m["convw"] = np.ascontiguousarray(cw.T.reshape(32, 128, 3).transpose(1, 0, 2))
        m["cosT"] = np.ascontiguousarray(cosT if hf == 0 else cosT[:, ::-1])
        m["sinT"] = np.ascontiguousarray(sinT if hf == 0 else sinT[:, ::-1])
        out.append(m)
    return out


def make_in_maps(inputs):
    x = np.asarray(inputs["x"], dtype=np.float32)
    sh = shared_maps(inputs)
    maps = []
    for c in range(N_CORES):
        b, hf = c // 2, c % 2
        xb = x[b]
        if hf == 1:
            xb = xb[::-1]
        m = dict(sh[hf])
        m["xT"] = fm(xb)
        maps.append(m)
    return maps


def unpack_out(outT, hf):
    o = np.asarray(outT).transpose(2, 1, 0).reshape(TOWN, D)
    return o[::-1] if hf == 1 else o


def kernel(**inputs):
    nc = build_program()
    maps = make_in_maps(inputs)
    res = run_bass_kernel_spmd(nc, maps, core_ids=list(range(N_CORES)))
    x = inputs["x"]
    out = np.zeros(x.shape, np.float32)
    for c in range(N_CORES):
        b, hf = c // 2, c % 2
        out[b, hf * TOWN:(hf + 1) * TOWN] = unpack_out(res.results[c]["outT"], hf)
    return out
```
